# Optimizing a Trainium2 kernel written in Bass

```python
import jax
import jax.numpy as jnp
from jax import lax
import numpy as np

D_MODEL = 1024
BATCH = 32
SEQ = 2048
DEPTH = 2

HEAD_DIM = 64
BLOCK = 128
A_HEADS = 8
A_KV_HEADS = 2
WINDOW = 128
B_HEADS = 8
FOX_BIAS_INIT = 3.0
C_HEADS = 8
IDX_HEADS = 4
IDX_DIM = 64
DSA_TOPK_MAX = 256
D_HEADS = 8
MLA_Q_RANK = 256
MLA_KV_RANK = 128
MLA_NOPE = 64
MLA_ROPE = 32
MLA_V = 64
ROPE_THETA = 10000.0
N_EXPERTS = 16
N_GROUPS = 4
EXPERTS_PER_GROUP = N_EXPERTS // N_GROUPS
TOPK_GROUPS = 1
TOPK_EXPERTS = 2
D_FF_EXPERT = 512
LN_EPS = 1e-5
RMS_EPS = 1e-6
ALPHA = (2 * DEPTH) ** 0.25
BETA = (8 * DEPTH) ** -0.25
N_EVEN = (DEPTH + 1) // 2
N_ODD = DEPTH // 2
MIX_WIDTH = A_HEADS * HEAD_DIM + B_HEADS * HEAD_DIM
EVEN_SIZES = (A_HEADS * HEAD_DIM, A_KV_HEADS * HEAD_DIM, A_KV_HEADS * HEAD_DIM,
              B_HEADS * HEAD_DIM, B_HEADS * HEAD_DIM, B_HEADS * HEAD_DIM, B_HEADS)
EVEN_VALUE_SLOTS = (2, 5)
ODD_SIZES = (C_HEADS * HEAD_DIM, HEAD_DIM, HEAD_DIM, IDX_HEADS * IDX_DIM, IDX_DIM, IDX_HEADS,
             MLA_Q_RANK, MLA_KV_RANK, MLA_ROPE)
ODD_VALUE_SLOTS = (2,)

kernel_name = 'hybrid_swa_fox_dsa_mla_grouped_moe_deepnorm'


def split_cols(h, sizes):
    out, start = [], 0
    for n in sizes:
        out.append(h[..., start:start + n])
        start += n
    return out


def layer_norm(x, g, b):
    xf = x.astype(jnp.float32)
    mu = jnp.mean(xf, -1, keepdims=True)
    var = jnp.mean(jnp.square(xf - mu), -1, keepdims=True)
    return ((xf - mu) * lax.rsqrt(var + LN_EPS)).astype(x.dtype) * g + b


def rms_norm(x, g):
    xf = x.astype(jnp.float32)
    return (xf * lax.rsqrt(jnp.mean(jnp.square(xf), -1, keepdims=True) + RMS_EPS)).astype(x.dtype) * g


def alibi_slopes(n):
    return 2.0 ** (-8.0 * jnp.arange(1, n + 1, dtype=jnp.float32) / n)


def rope(x, pos):
    half = x.shape[-1] // 2
    inv = ROPE_THETA ** (-jnp.arange(half, dtype=jnp.float32) / half)
    ang = pos.astype(jnp.float32)[:, None] * inv[None, :]
    shape = (1, x.shape[1]) + (1,) * (x.ndim - 3) + (half,)
    cos = jnp.cos(ang).reshape(shape).astype(x.dtype)
    sin = jnp.sin(ang).reshape(shape).astype(x.dtype)
    x1, x2 = x[..., :half], x[..., half:]
    return jnp.concatenate([x1 * cos - x2 * sin, x1 * sin + x2 * cos], axis=-1)


def sliding_window_sink_attention(q, k, v, sinks):
    B, S, Hq, d = q.shape
    Hkv = k.shape[2]
    G = Hq // Hkv
    nb = S // BLOCK
    qb = q.reshape(B, nb, BLOCK, Hkv, G, d)

    def band(a):
        a = a.reshape(B, nb, BLOCK, Hkv, d)
        prev = jnp.concatenate([jnp.zeros_like(a[:, :1]), a[:, :-1]], axis=1)
        return jnp.concatenate([prev, a], axis=2)

    k_band, v_band = band(k), band(v)
    logits = jnp.einsum('bnqkgd,bnskd->bnkgqs', qb, k_band,
                        preferred_element_type=jnp.float32) * d ** -0.5
    blk = jnp.arange(nb)[:, None, None]
    t = blk * BLOCK + jnp.arange(BLOCK)[None, :, None]
    s = (blk - 1) * BLOCK + jnp.arange(2 * BLOCK)[None, None, :]
    dist = t - s
    valid = (dist >= 0) & (dist < WINDOW) & (s >= 0)
    slopes = alibi_slopes(Hq).reshape(Hkv, G)
    logits = logits - slopes[:, :, None, None] * dist[:, None, None].astype(jnp.float32)
    logits = jnp.where(valid[:, None, None], logits, -jnp.inf)
    sink = jnp.broadcast_to(sinks.astype(jnp.float32).reshape(Hkv, G, 1, 1), logits.shape[:-1] + (1,))
    p = jax.nn.softmax(jnp.concatenate([logits, sink], axis=-1), axis=-1)[..., :-1].astype(v.dtype)
    out = jnp.einsum('bnkgqs,bnskd->bnqkgd', p, v_band)
    return out.reshape(B, S, Hq * d)


def causal_block_attention(q, k, v, log_forget=None):
    B, S, H, dk = q.shape
    dv = v.shape[-1]
    nb = S // BLOCK
    scale = dk ** -0.5
    pos = jnp.arange(S)
    cum = None if log_forget is None else jnp.cumsum(log_forget, axis=1).swapaxes(1, 2)

    def one_block(i):
        q_i = lax.dynamic_slice_in_dim(q, i * BLOCK, BLOCK, axis=1)
        t = i * BLOCK + jnp.arange(BLOCK)
        logits = jnp.einsum('bqhd,bshd->bhqs', q_i, k, preferred_element_type=jnp.float32) * scale
        if cum is not None:
            c_i = lax.dynamic_slice_in_dim(cum, i * BLOCK, BLOCK, axis=2)
            logits = logits + c_i[..., :, None] - cum[:, :, None, :]
        logits = jnp.where(pos[None, :] <= t[:, None], logits, -jnp.inf)
        p = jax.nn.softmax(logits, axis=-1).astype(v.dtype)
        return jnp.einsum('bhqs,bshd->bqhd', p, v)

    out = lax.map(one_block, jnp.arange(nb))
    return out.swapaxes(0, 1).reshape(B, S, H * dv)


def dsa_attention(q, k, v, q_idx, k_idx, w_idx):
    B, S, H, d = q.shape
    nb = S // BLOCK
    top_k = min(DSA_TOPK_MAX, S // 4)
    slopes = alibi_slopes(H)
    kv = jnp.concatenate([k, v], axis=-1)
    pos = jnp.arange(S)
    w_idx = w_idx.astype(jnp.float32) * IDX_HEADS ** -0.5

    def one_block(i):
        sl = lambda a: lax.dynamic_slice_in_dim(a, i * BLOCK, BLOCK, axis=1)
        q_i, qi_i, w_i = sl(q), sl(q_idx), sl(w_idx)
        t = i * BLOCK + jnp.arange(BLOCK)
        dots = jnp.einsum('bqhe,bse->bqhs', qi_i, k_idx,
                          preferred_element_type=jnp.float32) * IDX_DIM ** -0.5
        score = jnp.einsum('bqh,bqhs->bqs', w_i, jax.nn.relu(dots))
        score = jnp.where(pos[None, None, :] <= t[None, :, None], score, -jnp.inf)
        _, idx = lax.top_k(score, top_k)
        kv_sel = jax.vmap(lambda a, j: a[j])(kv, idx)
        k_sel, v_sel = kv_sel[..., :d], kv_sel[..., d:]
        dist = (t[None, :, None] - idx).astype(jnp.float32)
        logits = jnp.einsum('bqhd,bqkd->bhqk', q_i, k_sel,
                            preferred_element_type=jnp.float32) * d ** -0.5
        logits = logits - slopes[None, :, None, None] * dist[:, None]
        logits = jnp.where(dist[:, None] >= 0, logits, -jnp.inf)
        p = jax.nn.softmax(logits, axis=-1).astype(v.dtype)
        return jnp.einsum('bhqk,bqkd->bqhd', p, v_sel)

    out = lax.map(one_block, jnp.arange(nb))
    return out.swapaxes(0, 1).reshape(B, S, H * d)


def even_mixer(x, w_in, b_f, sinks, w_o):
    B, S, _ = x.shape
    qa, ka, va, qb, kb, vb, fg = split_cols(x @ w_in, EVEN_SIZES)
    qa = qa.reshape(B, S, A_HEADS, HEAD_DIM)
    ka = ka.reshape(B, S, A_KV_HEADS, HEAD_DIM)
    va = va.reshape(B, S, A_KV_HEADS, HEAD_DIM)
    qb = qb.reshape(B, S, B_HEADS, HEAD_DIM)
    kb = kb.reshape(B, S, B_HEADS, HEAD_DIM)
    vb = vb.reshape(B, S, B_HEADS, HEAD_DIM)
    log_f = jax.nn.log_sigmoid((fg + b_f).astype(jnp.float32))
    out_a = sliding_window_sink_attention(qa, ka, va, sinks)
    out_b = causal_block_attention(qb, kb, vb, log_f)
    return jnp.concatenate([out_a, out_b], axis=-1) @ w_o


def odd_mixer(x, w_in, q_norm_g, kv_norm_g, w_uq, w_ukv, w_o):
    B, S, _ = x.shape
    qc, kc, vc, qi, ki, wi, cq, ckv, kr = split_cols(x @ w_in, ODD_SIZES)
    out_c = dsa_attention(qc.reshape(B, S, C_HEADS, HEAD_DIM), kc, vc,
                          qi.reshape(B, S, IDX_HEADS, IDX_DIM), ki, wi)
    pos = jnp.arange(S)
    q = (rms_norm(cq, q_norm_g) @ w_uq).reshape(B, S, D_HEADS, MLA_NOPE + MLA_ROPE)
    kv = (rms_norm(ckv, kv_norm_g) @ w_ukv).reshape(B, S, D_HEADS, MLA_NOPE + MLA_V)
    q = jnp.concatenate([q[..., :MLA_NOPE], rope(q[..., MLA_NOPE:], pos)], axis=-1)
    k_rope = jnp.broadcast_to(rope(kr, pos)[:, :, None, :], (B, S, D_HEADS, MLA_ROPE))
    k = jnp.concatenate([kv[..., :MLA_NOPE], k_rope], axis=-1)
    out_d = causal_block_attention(q, k, kv[..., MLA_NOPE:])
    return jnp.concatenate([out_c, out_d], axis=-1) @ w_o


def routed_moe(x, router_w, router_b, w_gate, w_up, w_down):
    B, S, D = x.shape
    xt = x.reshape(B * S, D)
    aff = jax.nn.sigmoid((xt @ router_w).astype(jnp.float32))
    sel = aff + router_b.astype(jnp.float32)
    grp_score = lax.top_k(sel.reshape(-1, N_GROUPS, EXPERTS_PER_GROUP), 2)[0].sum(-1)
    _, g_idx = lax.top_k(grp_score, TOPK_GROUPS)
    g_mask = jax.nn.one_hot(g_idx, N_GROUPS, dtype=jnp.float32).sum(1) > 0
    e_mask = jnp.repeat(g_mask, EXPERTS_PER_GROUP, axis=1)
    _, e_idx = lax.top_k(jnp.where(e_mask, sel, -jnp.inf), TOPK_EXPERTS)
    w = jnp.take_along_axis(aff, e_idx, axis=1)
    w = w / jnp.sum(w, axis=-1, keepdims=True)
    gates = jnp.einsum('nk,nke->ne', w, jax.nn.one_hot(e_idx, N_EXPERTS, dtype=jnp.float32)).astype(x.dtype)
    y = jnp.zeros_like(xt)
    for e in range(N_EXPERTS):
        h = jax.nn.silu(xt @ w_gate[e]) * (xt @ w_up[e])
        y = y + gates[:, e:e + 1] * (h @ w_down[e])
    return y.reshape(B, S, D)


def _col_scale(sizes, value_slots):
    return jnp.concatenate([jnp.full((n,), BETA if i in value_slots else 1.0, jnp.float32)
                            for i, n in enumerate(sizes)])


def setup_inputs(seed: int = 0) -> dict:
    key = jax.random.key(seed)
    ks = jax.random.split(key, 20)
    nrm = lambda k, shape, scale: jax.random.normal(k, shape, jnp.float32) * scale
    even_in, odd_in = sum(EVEN_SIZES), sum(ODD_SIZES)
    ukv_scale = jnp.tile(jnp.concatenate([jnp.ones((MLA_NOPE,), jnp.float32),
                                          jnp.full((MLA_V,), BETA, jnp.float32)]), D_HEADS)
    return {
        'x': nrm(ks[0], (BATCH, SEQ, D_MODEL), 1.0),
        'even_w_in': nrm(ks[1], (N_EVEN, D_MODEL, even_in), D_MODEL ** -0.5) * _col_scale(EVEN_SIZES, EVEN_VALUE_SLOTS),
        'even_b_f': FOX_BIAS_INIT + nrm(ks[2], (N_EVEN, B_HEADS), 0.1),
        'even_sinks': nrm(ks[3], (N_EVEN, A_HEADS), 1.0),
        'odd_w_in': nrm(ks[4], (N_ODD, D_MODEL, odd_in), D_MODEL ** -0.5) * _col_scale(ODD_SIZES, ODD_VALUE_SLOTS),
        'odd_q_norm': 1.0 + nrm(ks[5], (N_ODD, MLA_Q_RANK), 0.02),
        'odd_kv_norm': 1.0 + nrm(ks[6], (N_ODD, MLA_KV_RANK), 0.02),
        'odd_w_uq': nrm(ks[7], (N_ODD, MLA_Q_RANK, D_HEADS * (MLA_NOPE + MLA_ROPE)), MLA_Q_RANK ** -0.5),
        'odd_w_ukv': nrm(ks[8], (N_ODD, MLA_KV_RANK, D_HEADS * (MLA_NOPE + MLA_V)), MLA_KV_RANK ** -0.5) * ukv_scale,
        'w_o': nrm(ks[9], (DEPTH, MIX_WIDTH, D_MODEL), MIX_WIDTH ** -0.5 * BETA),
        'ln_g': 1.0 + nrm(ks[10], (DEPTH, 2, D_MODEL), 0.02),
        'ln_b': nrm(ks[11], (DEPTH, 2, D_MODEL), 0.02),
        'router_w': nrm(ks[12], (D_MODEL, N_EXPERTS), D_MODEL ** -0.5),
        'router_b': nrm(ks[13], (N_EXPERTS,), 0.01),
        'moe_w_gate': nrm(ks[14], (DEPTH, N_EXPERTS, D_MODEL, D_FF_EXPERT), D_MODEL ** -0.5),
        'moe_w_up': nrm(ks[15], (DEPTH, N_EXPERTS, D_MODEL, D_FF_EXPERT), D_MODEL ** -0.5 * BETA),
        'moe_w_down': nrm(ks[16], (DEPTH, N_EXPERTS, D_FF_EXPERT, D_MODEL), D_FF_EXPERT ** -0.5 * BETA),
    }


def reference(x, even_w_in, even_b_f, even_sinks, odd_w_in, odd_q_norm, odd_kv_norm, odd_w_uq,
              odd_w_ukv, w_o, ln_g, ln_b, router_w, router_b, moe_w_gate, moe_w_up, moe_w_down):
    for l in range(DEPTH):
        j = l // 2
        if l % 2 == 0:
            mix = even_mixer(x, even_w_in[j], even_b_f[j], even_sinks[j], w_o[l])
        else:
            mix = odd_mixer(x, odd_w_in[j], odd_q_norm[j], odd_kv_norm[j], odd_w_uq[j], odd_w_ukv[j], w_o[l])
        x = layer_norm(ALPHA * x + mix, ln_g[l, 0], ln_b[l, 0])
        ffn = routed_moe(x, router_w, router_b, moe_w_gate[l], moe_w_up[l], moe_w_down[l])
        x = layer_norm(ALPHA * x + ffn, ln_g[l, 1], ln_b[l, 1])
    return x
```

```python
import contextlib
import os
import numpy as np
import concourse.bass as bass
import concourse.mybir as mybir
from concourse.bass_utils import run_bass_kernel_spmd

F32 = mybir.dt.float32
BF16 = mybir.dt.bfloat16
AF = mybir.ActivationFunctionType
ALU = mybir.AluOpType
AX = mybir.AxisListType

ENGS = ("pe", "act", "dve", "pool", "sp")

D = 1024
S = 2048
NT = 16
NE = 16
DFF = 512
ALPHA = 4.0 ** 0.25
LN_EPS = 1e-5
RMS_EPS = 1e-6
NEG = -30000.0


class Res:
    __slots__ = ("name", "w", "r")

    def __init__(self, name):
        self.name = name
        self.w = None
        self.r = []


class Op:
    __slots__ = ("eng", "idx", "fn", "deps", "is_dma", "ndma", "sem", "cnt", "needed", "_order")
    _ctr = [0]

    def __init__(self, eng, idx, fn, is_dma, ndma):
        self.eng = eng
        self.idx = idx
        self.fn = fn
        self.is_dma = is_dma
        self.ndma = ndma
        self.deps = []
        self.sem = None
        self.cnt = 0
        self.needed = False
        Op._ctr[0] += 1
        self._order = Op._ctr[0]


class Prog:
    NDMASEM = 24

    def __init__(self, nc):
        self.nc = nc
        self.ops = {e: [] for e in ENGS}
        self.waited = {e: {f: -1 for f in ENGS} for e in ENGS}
        self.dma_rr = 0
        self.dma_last = [None] * self.NDMASEM
        self.dma_waited = {e: set() for e in ENGS}
        self.all_dma = []

    def op(self, eng, fn, reads=(), writes=(), dma=0, selfdep=True):
        o = Op(eng, len(self.ops[eng]), fn, dma > 0, dma)
        deps = []
        for R in reads:
            if R.w is not None:
                deps.append(R.w)
        for R in writes:
            if R.w is not None:
                deps.append(R.w)
            deps.extend(R.r)
        if o.is_dma:
            half = self.NDMASEM // 2
            key = "rr_" + eng
            i_ = getattr(self, key, 0)
            setattr(self, key, i_ + 1)
            slot = (i_ % half) + (half if eng == "pool" else 0)
            prev = self.dma_last[slot]
            if prev is not None:
                deps.append(prev)
            self.dma_last[slot] = o
            o.sem = slot
            self.all_dma.append(o)
        best = {}
        for d in deps:
            if d is o:
                continue
            if d.is_dma:
                if id(d) in self.dma_waited[eng]:
                    continue
                best[("dma", id(d))] = d
            else:
                if d.eng == eng and (eng == "pe" or not selfdep):
                    continue
                if d.idx <= self.waited[eng][d.eng]:
                    continue
                k = ("e", d.eng)
                if k not in best or best[k].idx < d.idx:
                    best[k] = d
        for k, d in best.items():
            d.needed = True
            o.deps.append(d)
            if d.is_dma:
                self.dma_waited[eng].add(id(d))
            else:
                self.waited[eng][d.eng] = d.idx
        for R in reads:
            R.r.append(o)
        for R in writes:
            R.w = o
            R.r = []
        self.ops[eng].append(o)
        return o

    def barrier(self):
        lasts = []
        for e in ENGS:
            if e == "sp":
                continue
            for o_ in reversed(self.ops[e]):
                if o_.fn is not None and not o_.is_dma:
                    lasts.append(o_)
                    break
        dmas = list(self.all_dma)
        for e in ENGS:
            o = Op(e, len(self.ops[e]), None, False, 0)
            for d in lasts:
                if d.eng == e or d.is_dma:
                    continue
                if d.idx <= self.waited[e][d.eng]:
                    continue
                d.needed = True
                o.deps.append(d)
                self.waited[e][d.eng] = d.idx
            for d in dmas:
                if id(d) in self.dma_waited[e]:
                    continue
                d.needed = True
                o.deps.append(d)
                self.dma_waited[e].add(id(d))
            self.ops[e].append(o)
        self.all_dma = []

    def emit(self):
        nc = self.nc
        with contextlib.ExitStack() as st:
            esem = {e: st.enter_context(nc.semaphore("s_" + e)) for e in ENGS}
            dsem = [st.enter_context(nc.semaphore("d%d" % i)) for i in range(self.NDMASEM)]
            for e in ENGS:
                c = 0
                for o in self.ops[e]:
                    if o.is_dma:
                        continue
                    if o.needed:
                        c += 1
                        o.cnt = c
            dc = [0] * self.NDMASEM
            alld = []
            for e in ENGS:
                for o in self.ops[e]:
                    if o.is_dma:
                        alld.append(o)
            alld.sort(key=lambda o: o._order)
            for o in alld:
                dc[o.sem] += 16 * o.ndma
                o.cnt = dc[o.sem]
            block = st.enter_context(nc.Block())

            def run(eng_name, eng):
                for o in self.ops[eng_name]:
                    for d in o.deps:
                        if d.is_dma:
                            eng.wait_ge(dsem[d.sem], d.cnt)
                        else:
                            eng.wait_ge(esem[d.eng], d.cnt)
                    if o.fn is None:
                        continue
                    r = o.fn(eng)
                    if o.is_dma:
                        if not isinstance(r, (list, tuple)):
                            r = [r]
                        assert len(r) == o.ndma, (len(r), o.ndma)
                        for ins in r:
                            ins.then_inc(dsem[o.sem], 16)
                    elif o.needed:
                        if isinstance(r, (list, tuple)):
                            r = r[-1]
                        r.then_inc(esem[eng_name], 1)

            @block.tensor
            def _(eng):
                run("pe", eng)

            @block.scalar
            def _(eng):
                run("act", eng)

            @block.vector
            def _(eng):
                run("dve", eng)

            @block.gpsimd
            def _(eng):
                run("pool", eng)

            @block.sync
            def _(eng):
                run("sp", eng)


class K:
    pass


def _rr(lst, state, key):
    i = state.get(key, 0)
    state[key] = i + 1
    return lst[i % len(lst)]


def build(nseq, do_attn=True, do_moe=True, layers=(0, 1)):
    nc = bass.Bass("TRN2", target_bir_lowering=False)
    k = K()
    k.nc = nc
    k.nseq = nseq
    dt = nc.dram_tensor
    k.x = dt("x", [nseq, S, D], F32, kind="ExternalInput").ap()
    k.out = dt("out", [nseq, S, D], F32, kind="ExternalOutput").ap()
    k.even_w_in = dt("even_w_in", [D, 2312], F32, kind="ExternalInput").ap()
    k.even_b_f = dt("even_b_f", [8], F32, kind="ExternalInput").ap()
    k.even_sinks = dt("even_sinks", [8], F32, kind="ExternalInput").ap()
    k.odd_w_in = dt("odd_w_in", [D, 1380], F32, kind="ExternalInput").ap()
    k.odd_q_norm = dt("odd_q_norm", [256], F32, kind="ExternalInput").ap()
    k.odd_kv_norm = dt("odd_kv_norm", [128], F32, kind="ExternalInput").ap()
    k.odd_w_uq = dt("odd_w_uq", [256, 768], F32, kind="ExternalInput").ap()
    k.odd_w_ukv = dt("odd_w_ukv", [128, 1024], F32, kind="ExternalInput").ap()
    k.w_o = dt("w_o", [2, 1024, 1024], F32, kind="ExternalInput").ap()
    k.ln_g = dt("ln_g", [2, 2, D], F32, kind="ExternalInput").ap()
    k.ln_b = dt("ln_b", [2, 2, D], F32, kind="ExternalInput").ap()
    k.router_w = dt("router_w", [D, NE], F32, kind="ExternalInput").ap()
    k.router_b = dt("router_b", [NE], F32, kind="ExternalInput").ap()
    k.moe_w_gate = dt("moe_w_gate", [2, NE, D, DFF], F32, kind="ExternalInput").ap()
    k.moe_w_up = dt("moe_w_up", [2, NE, D, DFF], F32, kind="ExternalInput").ap()
    k.moe_w_down = dt("moe_w_down", [2, NE, DFF, D], F32, kind="ExternalInput").ap()
    k.c_attn = dt("c_attn", [128, CONST_COLS], F32, kind="ExternalInput").ap()

    with contextlib.ExitStack() as st:
        k.st = st
        k.P = Prog(nc)
        k.rr = {}
        _alloc_common(k)
        for s in range(nseq):
            _load_seq(k, s)
            for l in layers:
                if do_attn:
                    if l == 0:
                        _mixer_even(k)
                    else:
                        _mixer_odd(k)
                    if do_moe:
                        _moe_prefetch(k, l)
                    _layernorm(k, l, 0, last=(not do_moe and l == layers[-1]))
                if do_moe:
                    _moe(k, l)
                    _layernorm(k, l, 1, last=(l == layers[-1]))
            _store_seq(k, s)
        k.P.barrier()
        k.P.emit()
    return nc


C_CM = 0
C_MA = 128
C_TRI = 128 + 2048
C_CMQ = C_TRI + 128
C_ROPE = C_CMQ + 128
C_PK = C_ROPE + 4096
C_PQ = C_PK + 2048
C_PKD = C_PQ + 1024
C_PQD = C_PKD + 2048
CONST_COLS = C_PQD + 1024
SH_COLS = 22016


def _view(k, off_bytes, shape, dtype):
    n = 1
    for d_ in shape[1:]:
        n *= d_
    c0 = off_bytes // 4
    if dtype == BF16:
        ap = k.SH[:, c0:c0 + n // 2].bitcast(BF16)
    else:
        ap = k.SH[:, c0:c0 + n]
    if len(shape) == 2:
        return ap
    names = " ".join("d%d" % i for i in range(1, len(shape)))
    kw = {"d%d" % i: shape[i] for i in range(1, len(shape) - 1)}
    return ap.rearrange("p (%s) -> p %s" % (names, names), **kw)


def _T(k, name, shape, dtype):
    return k.st.enter_context(k.nc.sbuf_tensor(name, shape, dtype))


def _alloc_common(k):
    nc, P = k.nc, k.P
    k.X = _T(k, "X", [128, NT, D], F32)
    k.XT = _T(k, "XT", [128, 8, S], BF16)
    k.rX = [Res("X%d" % t) for t in range(NT)]
    k.rXT = [Res("XT%d" % t) for t in range(NT)]
    k.ident = _T(k, "ident", [128, 128], BF16)
    k.rident = Res("ident")
    k.eps = _T(k, "eps", [128, 2], F32)
    k.GB = _T(k, "GB", [128, 2, D], F32)
    k.rGB = Res("GB")
    k.xb = [_T(k, "xb%d" % i, [128, D], BF16) for i in range(2)]
    k.rxb = [Res("xb%d" % i) for i in range(2)]
    k.lnst = [_T(k, "lnst%d" % i, [128, 16], F32) for i in range(2)]
    k.rlnst = [Res("lnst%d" % i) for i in range(2)]
    k.ps = [k.st.enter_context(nc.psum_tensor("ps%d" % i, [128, 512], F32)) for i in range(8)]
    k.rps = [Res("ps%d" % i) for i in range(8)]
    P.op("pool", lambda e: e.memset(k.ident[:], 0.0), writes=[k.rident])
    P.op("pool", lambda e: e.affine_select(out=k.ident[:], in_=k.ident[:], pattern=[[-1, 128]],
                                          compare_op=ALU.not_equal, fill=1.0, base=0,
                                          channel_multiplier=1),
         reads=[k.rident], writes=[k.rident])
    P.op("pool", lambda e: e.memset(k.eps[:, 0:1], LN_EPS), writes=[k.rident])
    P.op("pool", lambda e: e.memset(k.eps[:, 1:2], RMS_EPS), writes=[k.rident])
    k.SH = _T(k, "SH", [128, SH_COLS], F32)
    k.wg = [_view(k, (24 * i) * 1024, [128, 8, DFF], BF16) for i in range(2)]
    k.wu = [_view(k, (24 * i + 8) * 1024, [128, 8, DFF], BF16) for i in range(2)]
    k.wd = [_view(k, (24 * i + 16) * 1024, [128, 4, D], BF16) for i in range(2)]
    k.rw = [Res("w%d" % i) for i in range(2)]
    k.hT = [_view(k, (48 + 4 * i) * 1024, [128, 4, 512], BF16) for i in range(2)]
    k.rhT = [Res("hT%d" % i) for i in range(2)]
    k.sg = [_view(k, (56 + 2 * i) * 1024, [128, 512], F32) for i in range(2)]
    k.rsg = [Res("sg%d" % i) for i in range(2)]
    k.rt = [_view(k, (60 + 2 * i) * 1024, [128, 512], F32) for i in range(6)]
    k.rrt = Res("rt")
    k.CMb = _T(k, "CMb", [128, 128], BF16)
    k.MAb = _T(k, "MAb", [128, 8, 2, 128], BF16)
    k.TRI = _T(k, "TRI", [128, 128], F32)
    k.ONESF = _T(k, "ONESF", [128, 128], F32)
    k.ONES2 = _T(k, "ONES2", [128, 2, 128], BF16)
    k.one1 = _T(k, "one1", [128, 1], F32)
    k.esink = _T(k, "esink", [128, 4], F32)
    k.bfb = _T(k, "bfb", [128, 8], F32)
    k.rconst = Res("const")
    k.ONESb = _T(k, "ONESb", [128, 128], BF16)
    k.gq = _T(k, "gq", [128, 2], F32)
    k.gkv = _T(k, "gkv", [128, 1], F32)
    k.nrm0 = _T(k, "nrm0", [128, 512], F32)
    k.nrm1 = _T(k, "nrm1", [128, 512], F32)
    DMA(k, "pool", [(k.CMb[:], k.c_attn[:, C_CM:C_CM + 128]),
                    (k.MAb[:].rearrange("p h r t -> p (h r t)"), k.c_attn[:, C_MA:C_MA + 2048])], [], [k.rconst])
    DMA(k, "sp", [(k.TRI[:], k.c_attn[:, C_TRI:C_TRI + 128]),
                  (k.esink[0:64, :], k.even_sinks[0:4].partition_broadcast(64)),
                  (k.esink[64:128, :], k.even_sinks[4:8].partition_broadcast(64)),
                  (k.bfb[:], k.even_b_f.partition_broadcast(128))], [], [k.rconst])
    OP(k, "pool", "memset", [], [k.rconst], ap=k.ONESF[:], constant=1.0)
    OP(k, "pool", "memset", [], [k.rconst], ap=k.ONESb[:], constant=1.0)
    DMA(k, "sp", [(k.gq[:, 0:1], k.odd_q_norm[0:128].rearrange("(p o) -> p o", o=1)),
                  (k.gq[:, 1:2], k.odd_q_norm[128:256].rearrange("(p o) -> p o", o=1)),
                  (k.gkv[:], k.odd_kv_norm.rearrange("(p o) -> p o", o=1))], [], [k.rconst])
    OP(k, "pool", "memset", [], [k.rconst], ap=k.one1[:], constant=1.0)
    OP(k, "pool", "memset", [], [k.rconst], ap=k.ONES2[:], constant=0.0)
    OP(k, "pool", "memset", [k.rconst], [k.rconst], ap=k.ONES2[:, 0, 0:64], constant=1.0)
    OP(k, "pool", "memset", [k.rconst], [k.rconst], ap=k.ONES2[:, 1, 64:128], constant=1.0)
    OP(k, "act", "activation", [k.rconst], [k.rconst], out=k.esink[:], in_=k.esink[:], func=AF.Exp)
    k.rwt = _T(k, "rwt", [128, 8, NE], BF16)
    k.rrwt = Res("rwt")
    k.rbt = _T(k, "rbt", [128, NE], F32)
    k.gates = _T(k, "gates", [128, NT * NE], F32)
    k.rgates = Res("gates")
    P.op("pool", lambda e: e.dma_start(out=k.rwt[:], in_=k.router_w.rearrange("(c p) e -> p c e", p=128)),
         writes=[k.rrwt], dma=1)
    P.op("sp", lambda e: e.dma_start(out=k.rbt[:], in_=k.router_b.partition_broadcast(128)),
         writes=[k.rrwt], dma=1)


def OP(k, eng, name, reads, writes, **kw):
    return k.P.op(eng, lambda e, kw=kw: getattr(e, name)(**kw), reads=reads, writes=writes)


def DMA(k, eng, pairs, reads, writes):
    return k.P.op(eng, lambda e, pairs=pairs: [e.dma_start(out=o, in_=i) for (o, i) in pairs],
                  reads=reads, writes=writes, dma=len(pairs))


def _nxt(k, key):
    i = k.rr.get(key, 0)
    k.rr[key] = i + 1
    return i


def _transpose_tile(k, t, scale):
    i = _nxt(k, "xb")
    xb, rxb = k.xb[i % 2], k.rxb[i % 2]
    OP(k, "act", "activation", [k.rX[t]], [rxb], out=xb[:], in_=k.X[:, t, :], func=AF.Copy, scale=scale)
    bank = 6 + (i % 2)
    pt = k.ps[bank][:].bitcast(BF16)
    for c in range(8):
        OP(k, "pe", "transpose", [rxb, k.rident], [k.rps[bank]], out=pt[:, c * 128:(c + 1) * 128],
           in_=xb[:, c * 128:(c + 1) * 128], identity=k.ident[:])
    OP(k, "dve", "tensor_copy", [k.rps[bank]], [k.rXT[t]], out=k.XT[:, :, t * 128:(t + 1) * 128],
       in_=pt.rearrange("p (c t) -> p c t", c=8))


def _load_seq(k, s):
    for t in range(NT):
        DMA(k, "sp", [(k.X[:, t, :], k.x[s, t * 128:(t + 1) * 128, :])], [], [k.rX[t]])
    for t in range(NT):
        _transpose_tile(k, t, 1.0)
        OP(k, "pool", "tensor_scalar", [k.rX[t]], [k.rX[t]], out=k.X[:, t, :], in0=k.X[:, t, :], scalar1=ALPHA,
           scalar2=None, op0=ALU.mult)


def _store_seq(k, s):
    for t in range(NT):
        DMA(k, "sp", [(k.out[s, t * 128:(t + 1) * 128, :], k.X[:, t, :])], [k.rX[t]], [])


def _layernorm(k, l, j, last):
    DMA(k, "sp", [(k.GB[:, 0, :], k.ln_g[l, j].partition_broadcast(128)),
                  (k.GB[:, 1, :], k.ln_b[l, j].partition_broadcast(128))], [], [k.rGB])
    if not last:
        OP(k, "pool", "tensor_scalar", [k.rGB], [k.rGB], out=k.GB[:], in0=k.GB[:], scalar1=ALPHA, scalar2=None,
           op0=ALU.mult)
    for t in range(NT):
        i = _nxt(k, "ln")
        stt, rst = k.lnst[i % 2], k.rlnst[i % 2]
        Xt = k.X[:, t, :]
        rX = k.rX[t]
        for h in range(2):
            OP(k, "dve", "bn_stats", [rX], [rst], out=stt[:, h * 6:(h + 1) * 6], in_=Xt[:, h * 512:(h + 1) * 512])
        OP(k, "dve", "bn_aggr", [rst], [rst], out=stt[:, 12:14], in_=stt[:, 0:12].rearrange("p (a b) -> p a b", a=2))
        OP(k, "act", "activation", [rst, k.rident], [rst], out=stt[:, 14:15], in_=stt[:, 13:14], func=AF.Sqrt,
           bias=k.eps[:, 0:1], scale=1.0)
        OP(k, "dve", "reciprocal", [rst], [rst], out=stt[:, 14:15], in_=stt[:, 14:15])
        OP(k, "dve", "scalar_tensor_tensor", [rst], [rst], out=stt[:, 15:16], in0=stt[:, 12:13], scalar=-1.0,
           in1=stt[:, 14:15], op0=ALU.mult, op1=ALU.mult)
        OP(k, "act", "activation", [rX, rst], [rX], out=Xt, in_=Xt, func=AF.Identity, bias=stt[:, 15:16],
           scale=stt[:, 14:15])
        OP(k, "dve", "tensor_tensor", [rX, k.rGB], [rX], out=Xt, in0=Xt, in1=k.GB[:, 0, :], op=ALU.mult)
        OP(k, "pool", "tensor_tensor", [rX, k.rGB], [rX], out=Xt, in0=Xt, in1=k.GB[:, 1, :], op=ALU.add)
        if not last:
            _transpose_tile(k, t, 1.0 / ALPHA)


def _load_expert(k, l, e, slot):
    DMA(k, "pool", [
        (k.wg[slot][:], k.moe_w_gate[l, e].rearrange("(c p) f -> p c f", p=128)),
        (k.wu[slot][:], k.moe_w_up[l, e].rearrange("(c p) f -> p c f", p=128)),
        (k.wd[slot][:], k.moe_w_down[l, e].rearrange("(c p) f -> p c f", p=128)),
    ], [], [k.rw[slot]])


def _router(k):
    bank = 5
    pr = k.ps[bank]
    for t in range(NT):
        for c in range(8):
            OP(k, "pe", "matmul", [k.rXT[t], k.rrwt], [k.rps[bank]], out=pr[:, t * 16:(t + 1) * 16],
               lhsT=k.XT[:, c, t * 128:(t + 1) * 128], rhs=k.rwt[:, c, :], start=(c == 0), stop=(c == 7))
    aff, sel, pairs, t3, t4, t5 = [x for x in k.rt]
    R = [k.rrt]
    N = NT * NE
    G = NT * 4
    OP(k, "act", "activation", [k.rps[bank]], R, out=aff[:, 0:N], in_=pr[:, 0:N], func=AF.Sigmoid)
    OP(k, "dve", "tensor_tensor", R + [k.rrwt], R, out=sel[:, 0:N].rearrange("p (t e) -> p t e", e=NE),
       in0=aff[:, 0:N].rearrange("p (t e) -> p t e", e=NE),
       in1=k.rbt[:].unsqueeze(1).broadcast_to([128, NT, NE]), op=ALU.add)
    sel4 = sel[:, 0:N].rearrange("p (g e) -> p g e", e=4)
    pv = pairs[:, 0:G * 6].rearrange("p (g q) -> p g q", q=6)
    pi = 0
    for a in range(4):
        for b in range(a + 1, 4):
            OP(k, "dve", "tensor_tensor", R, R, out=pv[:, :, pi:pi + 1], in0=sel4[:, :, a:a + 1],
               in1=sel4[:, :, b:b + 1], op=ALU.add)
            pi += 1
    gs = t3[:, 0:G]
    m1 = t3[:, G:2 * G]
    thr2 = t3[:, 2 * G:3 * G]
    gmax = t3[:, 3 * G:3 * G + NT]
    gmask = t3[:, 4 * G:5 * G]
    OP(k, "dve", "tensor_reduce", R, R, out=gs, in_=pv, axis=AX.X, op=ALU.max)
    OP(k, "dve", "tensor_reduce", R, R, out=m1, in_=sel4, axis=AX.X, op=ALU.max)
    em1 = t4[:, 0:N].rearrange("p (g e) -> p g e", e=4)
    selm = t5[:, 0:N].rearrange("p (g e) -> p g e", e=4)
    OP(k, "dve", "tensor_tensor", R, R, out=em1, in0=sel4, in1=m1.unsqueeze(2).broadcast_to([128, G, 4]),
       op=ALU.is_ge)
    OP(k, "dve", "scalar_tensor_tensor", R, R, out=selm, in0=em1, scalar=-1.0e9, in1=sel4, op0=ALU.mult, op1=ALU.add)
    OP(k, "dve", "tensor_reduce", R, R, out=thr2, in_=selm, axis=AX.X, op=ALU.max)
    OP(k, "dve", "tensor_reduce", R, R, out=gmax, in_=gs.rearrange("p (t g) -> p t g", g=4), axis=AX.X, op=ALU.max)
    OP(k, "dve", "tensor_tensor", R, R, out=gmask.rearrange("p (t g) -> p t g", g=4),
       in0=gs.rearrange("p (t g) -> p t g", g=4), in1=gmax.unsqueeze(2).broadcast_to([128, NT, 4]), op=ALU.is_ge)
    em = t4[:, 0:N].rearrange("p (g e) -> p g e", e=4)
    OP(k, "dve", "tensor_tensor", R, R, out=em, in0=sel4, in1=thr2.unsqueeze(2).broadcast_to([128, G, 4]),
       op=ALU.is_ge)
    OP(k, "dve", "tensor_tensor", R, R, out=em, in0=em, in1=gmask.unsqueeze(2).broadcast_to([128, G, 4]),
       op=ALU.mult)
    ga = t5[:, 0:N]
    OP(k, "dve", "tensor_tensor", R, R, out=ga, in0=t4[:, 0:N], in1=aff[:, 0:N], op=ALU.mult)
    den = t5[:, N:N + NT]
    OP(k, "dve", "tensor_reduce", R, R, out=den, in_=ga.rearrange("p (t e) -> p t e", e=NE), axis=AX.X, op=ALU.add)
    OP(k, "dve", "reciprocal", R, R, out=den, in_=den)
    OP(k, "dve", "tensor_tensor", R, [k.rgates], out=k.gates[:].rearrange("p (t e) -> p t e", e=NE),
       in0=ga.rearrange("p (t e) -> p t e", e=NE), in1=den.unsqueeze(2).broadcast_to([128, NT, NE]), op=ALU.mult)


def _moe_prefetch(k, l):
    _load_expert(k, l, 0, 0)
    _load_expert(k, l, 1, 1)
    k.moe_pref = True


def _moe(k, l):
    _router(k)
    if not getattr(k, "moe_pref", False):
        _moe_prefetch(k, l)
    k.moe_pref = False
    items = []
    for e in range(NE):
        slot = e % 2
        wg, wu, wd, rw = k.wg[slot], k.wu[slot], k.wd[slot], k.rw[slot]
        for tc in range(4):
            st = {}

            def gu(e=e, tc=tc, st=st, wg=wg, wu=wu, rw=rw):
                hi = _nxt(k, "hT")
                hT, rhT = k.hT[hi % 2], k.rhT[hi % 2]
                st["hT"], st["rhT"] = hT, rhT
                cols = slice(tc * 512, (tc + 1) * 512)
                rxt = [k.rXT[tc * 4 + i] for i in range(4)]
                for ff in range(4):
                    gi = _nxt(k, "gu")
                    bg, bu = (gi % 2) * 2, (gi % 2) * 2 + 1
                    for c in range(8):
                        OP(k, "pe", "matmul", rxt + [rw], [k.rps[bg]], out=k.ps[bg][:],
                           lhsT=wg[:, c, ff * 128:(ff + 1) * 128], rhs=k.XT[:, c, cols], start=(c == 0), stop=(c == 7))
                    for c in range(8):
                        OP(k, "pe", "matmul", rxt + [rw], [k.rps[bu]], out=k.ps[bu][:],
                           lhsT=wu[:, c, ff * 128:(ff + 1) * 128], rhs=k.XT[:, c, cols], start=(c == 0), stop=(c == 7))
                    sg, rsg = k.sg[gi % 2], k.rsg[gi % 2]
                    OP(k, "act", "activation", [k.rps[bg]], [rsg], out=sg[:], in_=k.ps[bg][:], func=AF.Silu)
                    OP(k, "dve", "tensor_tensor", [rsg, k.rps[bu]], [rhT], out=hT[:, ff, :], in0=sg[:], in1=k.ps[bu][:],
                       op=ALU.mult)

            def down(e=e, tc=tc, st=st, wd=wd, rw=rw):
                hT, rhT = st["hT"], st["rhT"]
                for tt in range(4):
                    t = tc * 4 + tt
                    for half in range(2):
                        yi = _nxt(k, "y")
                        by = 4 + (yi % 4)
                        for ff in range(4):
                            OP(k, "pe", "matmul", [rhT, rw], [k.rps[by]], out=k.ps[by][:],
                               lhsT=hT[:, ff, tt * 128:(tt + 1) * 128], rhs=wd[:, ff, half * 512:(half + 1) * 512],
                               start=(ff == 0), stop=(ff == 3))
                        Xs = k.X[:, t, half * 512:(half + 1) * 512]
                        OP(k, "dve", "scalar_tensor_tensor", [k.rps[by], k.rgates, k.rX[t]], [k.rX[t]], out=Xs,
                           in0=k.ps[by][:], scalar=k.gates[:, t * NE + e:t * NE + e + 1], in1=Xs, op0=ALU.mult,
                           op1=ALU.add)
                if tc == 3 and e + 2 < NE:
                    _load_expert(k, l, e + 2, e % 2)
            items.append((gu, down))
    _pipeline(items)


def _proj_feat(k, W, rW, wc0, dst, rdst, scale, nrow=128, src=None, evac=None):
    for tcn in range(4):
        bi = _nxt(k, "pj")
        bank = 6 + (bi % 2)
        if src is None:
            sap, rs, nch = k.XT, [k.rXT[tcn * 4 + i] for i in range(4)], 8
        else:
            sap, rs, nch = src
        for c in range(nch):
            OP(k, "pe", "matmul", rs + [rW], [k.rps[bank]], out=k.ps[bank][0:nrow, :],
               lhsT=W[:, c, wc0:wc0 + nrow], rhs=sap[:, c, tcn * 512:(tcn + 1) * 512], start=(c == 0), stop=(c == nch - 1))
        if evac is not None:
            evac(tcn, bank)
        else:
            OP(k, "act", "activation", [k.rps[bank]], [rdst], out=dst[0:nrow, tcn * 512:(tcn + 1) * 512],
               in_=k.ps[bank][0:nrow, :], func=AF.Copy, scale=scale)


def _proj_tok(k, W, rW, wc0, ncols, t, outs, routs):
    bi = _nxt(k, "pj")
    bank = 6 + (bi % 2)
    for c in range(8):
        OP(k, "pe", "matmul", [k.rXT[t], rW], [k.rps[bank]], out=k.ps[bank][:, 0:ncols],
           lhsT=k.XT[:, c, t * 128:(t + 1) * 128], rhs=W[:, c, wc0:wc0 + ncols], start=(c == 0), stop=(c == 7))
    for (dst, lo, hi) in outs:
        OP(k, "dve", "tensor_copy", [k.rps[bank]], routs, out=dst, in_=k.ps[bank][:, lo:hi])


def _wslice(k, wsrc, col_ranges, W, rW):
    pairs = []
    off = 0
    v = wsrc.rearrange("(c p) f -> p c f", p=128)
    for cr in col_ranges:
        if len(cr) == 3:
            off, c0, n = cr
        else:
            c0, n = cr
        pairs.append((W[:, :, off:off + n], v[:, :, c0:c0 + n]))
        off += n
    DMA(k, "pool", pairs, [], [rW])


def _attn_views(k):
    a = K()
    a.attnT = _view(k, 0, [128, 8, S], BF16)
    a.rattn = [Res("attn%d" % c) for c in range(8)]
    a.QP = [_view(k, (32 + 4 * i) * 1024, [128, S], BF16) for i in range(2)]
    a.rQP = [Res("QP%d" % i) for i in range(2)]
    a.KP = [_view(k, (40 + 4 * i) * 1024, [128, S], BF16) for i in range(2)]
    a.rKP = [Res("KP%d" % i) for i in range(2)]
    a.VP = _view(k, 48 * 1024, [128, NT, 2, 128], BF16)
    a.rVP = Res("VP")
    a.ET = [_view(k, (56 + i) * 1024, [128, 512], BF16) for i in range(4)]
    a.rET = [Res("ET%d" % i) for i in range(4)]
    a.W = _view(k, 60 * 1024, [128, 8, 384], BF16)
    a.rW = Res("Wsl")
    a.FB = _view(k, 66 * 1024, [128, 4, NT, 8], F32)
    a.cp = _view(k, 68 * 1024, [128, NT, 8], F32)
    a.Tp = _view(k, 68 * 1024 + 512, [128, NT + 1, 8], F32)
    a.TT = _view(k, 69 * 1024 + 128, [128, NT, 8], F32)
    a.zt = _view(k, 70 * 1024, [128, NT, 8], F32)
    a.nrm = [_view(k, 32 * 1024 + 0, [128, 512], F32)]
    a.nrm = [k.nrm0, k.nrm1]
    a.rnrm = [Res("nrm0"), Res("nrm1")]
    a.rfox = Res("fox")
    a.WO = _view(k, 32 * 1024, [128, 8, D], BF16)
    a.rWO = Res("WO")
    a.racc = Res("acc")
    a.rden = Res("den")
    return a


def _wo_apply(k, a, l, row_map):
    k.P.barrier()
    pairs = []
    for c in range(8):
        for (plo, n, r0) in row_map[c]:
            pairs.append((a.WO[plo:plo + n, c, :], k.w_o[l, r0:r0 + n, :]))
    DMA(k, "pool", pairs, [], [a.rWO])
    for t in range(NT):
        for half in range(2):
            bi = _nxt(k, "pj")
            bank = 6 + (bi % 2)
            for c in range(8):
                OP(k, "pe", "matmul", [a.rattn[c], a.rWO], [k.rps[bank]], out=k.ps[bank][:],
                   lhsT=a.attnT[:, c, t * 128:(t + 1) * 128], rhs=a.WO[:, c, half * 512:(half + 1) * 512],
                   start=(c == 0), stop=(c == 7))
            Xs = k.X[:, t, half * 512:(half + 1) * 512]
            OP(k, "dve", "tensor_tensor", [k.rps[bank], k.rX[t]], [k.rX[t]], out=Xs, in0=k.ps[bank][:], in1=Xs,
               op=ALU.add)
    k.P.barrier()


def _pipeline(items):
    if not items:
        return
    items[0][0]()
    for n_ in range(len(items)):
        if n_ + 1 < len(items):
            items[n_ + 1][0]()
        items[n_][1]()


def _causal_pair(k, a, chunk, QP, rQP, KP, rKP, VP, rVP, bias_fn, krows=64, extra=None):
    items = []
    for c in range(4):
        nj = 4 * c + 4
        for j in range(nj):
            st = {}

            def qk(c=c, j=j, st=st):
                a0 = max(0, j - 4 * c) * 128
                diag = j >= 4 * c
                ets = []
                for hd in range(2):
                    lb = _nxt(k, "lt") % 4
                    LT = k.ps[lb]
                    kk = KP[64 * hd:64 * hd + krows, j * 128:(j + 1) * 128]
                    qbase = c * 512
                    if extra is not None:
                        QR, rQR, KR, rKR = extra
                        OP(k, "pe", "matmul", [rQR, rKR], [k.rps[lb]], out=LT[:, a0:512],
                           lhsT=KR[64 * hd:64 * hd + 64, j * 128:(j + 1) * 128],
                           rhs=QR[64 * hd:64 * hd + 64, qbase + a0:qbase + 512], start=True, stop=False)
                    st0 = extra is None
                    if diag:
                        OP(k, "pe", "matmul", [rQP, rKP], [k.rps[lb]], out=LT[:, a0:a0 + 128], lhsT=kk,
                           rhs=QP[64 * hd:64 * hd + krows, qbase + a0:qbase + a0 + 128], start=st0, stop=False)
                        if st0:
                            OP(k, "pe", "matmul", [k.rconst, k.rident], [k.rps[lb]], out=LT[:, a0:a0 + 128], lhsT=k.ident[:],
                               rhs=k.CMb[:], start=False, stop=True)
                        if a0 + 128 < 512:
                            OP(k, "pe", "matmul", [rQP, rKP], [k.rps[lb]], out=LT[:, a0 + 128:512], lhsT=kk,
                               rhs=QP[64 * hd:64 * hd + krows, qbase + a0 + 128:qbase + 512], start=st0, stop=st0)
                        if not st0:
                            OP(k, "pe", "matmul", [k.rconst, k.rident], [k.rps[lb]], out=LT[:, a0:a0 + 128], lhsT=k.ident[:],
                               rhs=k.CMb[:], start=False, stop=True)
                    else:
                        OP(k, "pe", "matmul", [rQP, rKP], [k.rps[lb]], out=LT[:, 0:512], lhsT=kk,
                           rhs=QP[64 * hd:64 * hd + krows, qbase:qbase + 512], start=st0, stop=True)
                    ei = _nxt(k, "et") % 4
                    ET, rET = a.ET[ei], a.rET[ei]
                    b = bias_fn(c, j, hd) if bias_fn is not None else None
                    if b is not None:
                        OP(k, "act", "activation", [k.rps[lb], a.rfox], [rET], out=ET[:, a0:512], in_=LT[:, a0:512],
                           func=AF.Exp, bias=b, scale=1.0)
                    else:
                        OP(k, "act", "activation", [k.rps[lb]], [rET], out=ET[:, a0:512], in_=LT[:, a0:512], func=AF.Exp)
                    ets.append((ET, rET))
                st["ets"] = ets
                st["a0"] = a0

            def pv(c=c, j=j, nj=nj, st=st):
                a0 = st["a0"]
                for hd in range(2):
                    ET, rET = st["ets"][hd]
                    first = (j == 0 and hd == 0)
                    last = (j == nj - 1 and hd == 1)
                    OP(k, "pe", "matmul", [rET, rVP], [a.racc], out=k.ps[4][:, a0:512], lhsT=VP[:, j, hd, :],
                       rhs=ET[:, a0:512], start=first, stop=last)
                    OP(k, "pe", "matmul", [rET, k.rconst], [a.rden], out=k.ps[5][:, a0:512], lhsT=k.ONES2[:, hd, :],
                       rhs=ET[:, a0:512], start=first, stop=last)
                if j == nj - 1:
                    ni = _nxt(k, "nrm") % 2
                    nrm, rn = a.nrm[ni], a.rnrm[ni]
                    OP(k, "dve", "reciprocal", [a.rden], [rn], out=nrm[:], in_=k.ps[5][:])
                    OP(k, "dve", "tensor_tensor", [a.racc, rn], [a.rattn[chunk]],
                       out=a.attnT[:, chunk, c * 512:(c + 1) * 512], in0=k.ps[4][:], in1=nrm[:], op=ALU.mult)
            items.append((qk, pv))
    _pipeline(items)


def _mixer_even(k):
    k.P.barrier()
    a = _attn_views(k)
    wsrc = k.even_w_in
    Wb = [a.W, _view(k, 72 * 1024, [128, 8, 384], BF16)]
    rWb = [a.rW, Res("Wsl2")]
    specs = [[(2304, 8)], [(512, 128), (640, 128)]]
    specs += [[(64 * p_, 64), (64 * (p_ + 4), 64)] for p_ in range(4)]
    specs += [[(768 + 128 * p_, 128), (1280 + 128 * p_, 128), (1792 + 128 * p_, 128)] for p_ in range(4)]
    wst = {"n": 0}

    def take_w():
        n_ = wst["n"]
        if n_ == 0:
            _wslice(k, wsrc, specs[0], Wb[0], rWb[0])
        if n_ + 1 < len(specs):
            _wslice(k, wsrc, specs[n_ + 1], Wb[(n_ + 1) % 2], rWb[(n_ + 1) % 2])
        wst["n"] = n_ + 1
        return Wb[n_ % 2], rWb[n_ % 2]
    Wc, rWc = take_w()
    bank = 6
    for t in range(NT):
        for c in range(8):
            OP(k, "pe", "matmul", [k.rXT[t], rWc], [k.rps[bank]], out=k.ps[bank][:, t * 8:(t + 1) * 8],
               lhsT=k.XT[:, c, t * 128:(t + 1) * 128], rhs=Wc[:, c, 0:8], start=(c == 0), stop=(c == 7))
    R = [a.rfox]
    OP(k, "dve", "tensor_tensor", [k.rps[bank], k.rconst], R, out=a.zt, in0=k.ps[bank][:, 0:128].rearrange("p (t h) -> p t h", h=8),
       in1=k.bfb[:].unsqueeze(1).broadcast_to([128, NT, 8]), op=ALU.add)
    OP(k, "act", "activation", R, R, out=a.zt, in_=a.zt, func=AF.Exp, scale=-1.0)
    OP(k, "act", "activation", R + [k.rconst], R, out=a.zt, in_=a.zt, func=AF.Ln, bias=k.one1[:], scale=1.0)
    ztf = a.zt.rearrange("p t h -> p (t h)")
    b2 = 7
    OP(k, "pe", "matmul", R + [k.rconst], [k.rps[b2]], out=k.ps[b2][:, 0:128], lhsT=k.TRI[:], rhs=ztf, start=True, stop=True)
    OP(k, "pe", "matmul", R + [k.rconst], [k.rps[b2]], out=k.ps[b2][:, 128:256], lhsT=k.ONESF[:], rhs=ztf, start=True, stop=True)
    OP(k, "dve", "tensor_copy", [k.rps[b2]], R, out=a.TT, in_=k.ps[b2][:, 128:256].rearrange("p (t h) -> p t h", h=8))
    OP(k, "dve", "memset", [], R, ap=a.Tp[:, 0, :], constant=0.0)
    for j in range(NT):
        OP(k, "dve", "tensor_tensor", R, R, out=a.Tp[:, j + 1, :], in0=a.Tp[:, j, :], in1=a.TT[:, j, :], op=ALU.add)
    OP(k, "dve", "tensor_tensor", [k.rps[b2]] + R, R, out=a.cp, in0=k.ps[b2][:, 0:128].rearrange("p (t h) -> p t h", h=8),
       in1=a.Tp[:, 0:NT, :], op=ALU.add)
    for c in range(4):
        OP(k, "dve", "tensor_tensor", R, R, out=a.FB[:, c, :, :], in0=a.cp,
           in1=a.Tp[:, 4 * c, :].unsqueeze(1).broadcast_to([128, NT, 8]), op=ALU.subtract)
    dbg = int(os.environ.get('KDBG', '9'))
    OP(k, "pool", "memset", [], [a.rVP], ap=a.VP, constant=0.0)
    Wc, rWc = take_w()
    K2, rK2 = a.KP[0], a.rKP[0]
    _proj_feat(k, Wc, rWc, 0, K2, rK2, 1.0)
    for t in range(NT):
        _proj_tok(k, Wc, rWc, 128, 128, t, [(a.VP[:, t, 0, 0:64], 0, 64), (a.VP[:, t, 1, 64:128], 64, 128)], [a.rVP])
    for p in range(4 if dbg >= 2 else 0):
        Wc, rWc = take_w()
        QP, rQP = a.QP[p % 2], a.rQP[p % 2]
        _proj_feat(k, Wc, rWc, 0, QP, rQP, 0.125)
        items = []
        for i in range(NT):
            st = {}

            def qk(i=i, st=st, p=p, QP=QP, rQP=rQP):
                lb = _nxt(k, "lt") % 4
                LT = k.ps[lb]
                jl = [i] if i == 0 else [i - 1, i]
                idx = 0
                for jj in jl:
                    rel = i - jj
                    for hd in range(2):
                        h = p + 4 * hd
                        o = LT[:, idx * 128:(idx + 1) * 128]
                        OP(k, "pe", "matmul", [rQP, rK2], [k.rps[lb]], out=o, lhsT=K2[64 * hd:64 * hd + 64, jj * 128:(jj + 1) * 128],
                           rhs=QP[64 * hd:64 * hd + 64, i * 128:(i + 1) * 128], start=True, stop=False)
                        OP(k, "pe", "matmul", [k.rconst, k.rident], [k.rps[lb]], out=o, lhsT=k.ident[:], rhs=k.MAb[:, h, rel, :],
                           start=False, stop=True)
                        idx += 1
                n = idx * 128
                ei = _nxt(k, "et") % 4
                ET, rET = a.ET[ei], a.rET[ei]
                OP(k, "act", "activation", [k.rps[lb]], [rET], out=ET[:, 0:n], in_=LT[:, 0:n], func=AF.Exp)
                st["ET"], st["rET"], st["jl"] = ET, rET, jl

            def pv(i=i, st=st, p=p):
                ET, rET, jl = st["ET"], st["rET"], st["jl"]
                sl = (i % 4) * 128
                idx = 0
                for jj in jl:
                    for hd in range(2):
                        first, last = (idx == 0), (idx == 2 * len(jl) - 1)
                        OP(k, "pe", "matmul", [rET, a.rVP], [a.racc], out=k.ps[4][:, 0:128], lhsT=a.VP[:, jj, hd, :],
                           rhs=ET[:, idx * 128:(idx + 1) * 128], start=first, stop=last)
                        OP(k, "pe", "matmul", [rET, k.rconst], [a.rden], out=k.ps[5][:, 0:128], lhsT=k.ONES2[:, hd, :],
                           rhs=ET[:, idx * 128:(idx + 1) * 128], start=first, stop=last)
                        idx += 1
                ni = _nxt(k, "nrm") % 2
                nrm, rn = a.nrm[ni], a.rnrm[ni]
                OP(k, "dve", "tensor_scalar", [a.rden, k.rconst], [rn], out=nrm[:, 0:128], in0=k.ps[5][:, 0:128],
                   scalar1=k.esink[:, p:p + 1], scalar2=None, op0=ALU.add)
                OP(k, "dve", "reciprocal", [rn], [rn], out=nrm[:, 0:128], in_=nrm[:, 0:128])
                OP(k, "dve", "tensor_tensor", [a.racc, rn], [a.rattn[p]], out=a.attnT[:, p, i * 128:(i + 1) * 128],
                   in0=k.ps[4][:, 0:128], in1=nrm[:, 0:128], op=ALU.mult)
            items.append((qk, pv))
        _pipeline(items)
    for p in range(4 if dbg >= 3 else 0):
        Wc, rWc = take_w()
        QP, rQP = a.QP[p % 2], a.rQP[p % 2]
        KP, rKP = a.KP[p % 2], a.rKP[p % 2]
        _proj_feat(k, Wc, rWc, 0, QP, rQP, 0.125)
        _proj_feat(k, Wc, rWc, 128, KP, rKP, 1.0)
        for t in range(NT):
            _proj_tok(k, Wc, rWc, 256, 128, t, [(a.VP[:, t, 0, 0:64], 0, 64), (a.VP[:, t, 1, 64:128], 64, 128)], [a.rVP])
        _causal_pair(k, a, 4 + p, QP, rQP, KP, rKP, a.VP, a.rVP,
                     lambda c, j, hd, p=p: a.FB[:, c, j, 2 * p + hd:2 * p + hd + 1])
    row_map = {}
    for p in range(4):
        row_map[p] = [(0, 64, 64 * p), (64, 64, 64 * (p + 4))]
        row_map[4 + p] = [(0, 128, 512 + 128 * p)]
    _wo_apply(k, a, 0, row_map)


def _rmsnorm_feat(k, a, src, rsrc, nch, gt, dst, rdst, RS, rRS):
    for tcn in range(4):
        cols = slice(tcn * 512, (tcn + 1) * 512)
        bi = _nxt(k, "pj")
        bank = 6 + (bi % 2)
        for c in range(nch):
            ei = _nxt(k, "et") % 4
            OP(k, "dve", "tensor_tensor", rsrc, [a.rET[ei]], out=a.ET[ei][:], in0=src[:, c, cols], in1=src[:, c, cols],
               op=ALU.mult)
            OP(k, "pe", "matmul", [a.rET[ei], k.rconst], [k.rps[bank]], out=k.ps[bank][:], lhsT=k.ONESb[:], rhs=a.ET[ei][:],
               start=(c == 0), stop=(c == nch - 1))
        OP(k, "act", "activation", [k.rps[bank], k.rident], [rRS], out=RS[:, cols], in_=k.ps[bank][:], func=AF.Sqrt,
           bias=k.eps[:, 1:2], scale=1.0 / (128.0 * nch))
        OP(k, "dve", "reciprocal", [rRS], [rRS], out=RS[:, cols], in_=RS[:, cols])
        for c in range(nch):
            OP(k, "dve", "scalar_tensor_tensor", rsrc + [rRS, k.rconst], [rdst], out=dst[:, c, cols], in0=src[:, c, cols],
               scalar=gt[:, c:c + 1], in1=RS[:, cols], op0=ALU.mult, op1=ALU.mult)


def _rope_evac(k, a, bankA, bankB, dst, rdst, cols, scale):
    t1, r1 = a.nrm[0], a.rnrm[0]
    t2, r2 = a.nrm[1], a.rnrm[1]
    OP(k, "dve", "scalar_tensor_tensor", [k.rps[bankA], a.rrope], [r1], out=t1[:, :], in0=k.ps[bankA][:, :], scalar=scale,
       in1=a.ROPE[:, 0, cols], op0=ALU.mult, op1=ALU.mult)
    OP(k, "dve", "scalar_tensor_tensor", [k.rps[bankB], a.rrope], [r2], out=t2[:, :], in0=k.ps[bankB][:, :], scalar=scale,
       in1=a.ROPE[:, 1, cols], op0=ALU.mult, op1=ALU.mult)
    OP(k, "pool", "tensor_tensor", [r1, r2], [rdst], out=dst[:, cols], in0=t1[:, :], in1=t2[:, :], op=ALU.add)


def _rope_proj(k, a, W, rW, cA, cB, src, dst, rdst, scale):
    sap, rs, nch = src
    for tcn in range(4):
        cols = slice(tcn * 512, (tcn + 1) * 512)
        rr_ = rs if rs is not None else [k.rXT[tcn * 4 + i] for i in range(4)]
        for (bank, c0) in ((6, cA), (7, cB)):
            for c in range(nch):
                OP(k, "pe", "matmul", rr_ + [rW], [k.rps[bank]], out=k.ps[bank][:, :], lhsT=W[:, c, c0:c0 + 128],
                   rhs=sap[:, c, cols], start=(c == 0), stop=(c == nch - 1))
        _rope_evac(k, a, 6, 7, dst, rdst, cols, scale)


def _mixer_odd(k):
    k.P.barrier()
    a = _attn_views(k)
    wsrc = k.odd_w_in
    KB = 1024
    a.CQN = _view(k, 66 * KB, [128, 2, S], BF16)
    a.rCQN = Res("CQN")
    a.CKVN = _view(k, 74 * KB, [128, 1, S], BF16)
    a.rCKVN = Res("CKVN")
    a.ROPE = _view(k, 78 * KB, [128, 2, S], BF16)
    a.rrope = Res("rope")
    a.QR = a.QP[1]
    a.rQR = a.rQP[1]
    a.KR = a.KP[1]
    a.rKR = a.rKP[1]
    RS = a.VP.rearrange("p t v d -> p (t v d)").bitcast(F32)
    gq = k.gq
    DMA(k, "pool", [(a.ROPE.rearrange("p a s -> p (a s)"), k.c_attn[:, C_ROPE:C_ROPE + 4096])], [], [a.rrope])
    raw = _view(k, 32 * KB, [128, 2, S], BF16)
    rraw = a.rQP[0]
    _wslice(k, wsrc, [(964, 256), (1220, 128)], a.W, a.rW)
    _proj_feat(k, a.W, a.rW, 0, raw[:, 0, :], a.rQP[0], 1.0)
    _proj_feat(k, a.W, a.rW, 128, raw[:, 1, :], a.rQP[1], 1.0)
    rawkv = _view(k, 40 * KB, [128, 1, S], BF16)
    _proj_feat(k, a.W, a.rW, 256, rawkv[:, 0, :], a.rKP[0], 1.0)
    class _RR:
        pass
    for tcn in range(4):
        pass
    dbg = int(os.environ.get('KDBG', '9'))
    if dbg >= 2:
        _rmsnorm_feat(k, a, raw, [a.rQP[0], a.rQP[1]], 2, k.gq, a.CQN, a.rCQN, RS, a.rVP)
        _rmsnorm_feat(k, a, rawkv, [a.rKP[0]], 1, k.gkv, a.CKVN, a.rCKVN, RS, a.rVP)
    OP(k, "pool", "memset", [a.rW], [a.rW], ap=a.W[:, :, 0:256], constant=0.0)
    _wslice(k, wsrc, [(0, 1348, 32), (64, 1348, 32), (128, 1364, 16), (144, 1348, 16), (192, 1364, 16), (208, 1348, 16)],
            a.W, a.rW)
    if dbg >= 3:
        _rope_proj(k, a, a.W, a.rW, 0, 128, (k.XT, None, 8), a.KR, a.rKR, 1.0)
    OP(k, "pool", "memset", [a.rVP], [a.rVP], ap=a.VP, constant=0.0)
    WQb = [_view(k, 60 * KB, [128, 2, 384], BF16), _view(k, 62 * KB, [128, 2, 384], BF16)]
    WKVb = [_view(k, 61 * KB + 512, [128, 1, 256], BF16), _view(k, 63 * KB + 512, [128, 1, 256], BF16)]
    rBb = [Res("WB0"), Res("WB1")]
    sc = 96.0 ** -0.5

    def load_pair(p):
        b_ = p % 2
        h0, h1 = 2 * p, 2 * p + 1
        extra = [a.rW] if p < 2 else []
        OP(k, "pool", "memset", [], [rBb[b_]] + extra, ap=WQb[b_][:, :, 128:384], constant=0.0)
        _wslice(k, k.odd_w_uq, [(0, h0 * 96, 64), (64, h1 * 96, 64), (128, h0 * 96 + 64, 32), (192, h1 * 96 + 64, 32),
                                (256, h0 * 96 + 80, 16), (272, h0 * 96 + 64, 16), (320, h1 * 96 + 80, 16),
                                (336, h1 * 96 + 64, 16)], WQb[b_], rBb[b_])
        _wslice(k, k.odd_w_ukv, [(h0 * 128, 64), (h1 * 128, 64), (h0 * 128 + 64, 64), (h1 * 128 + 64, 64)], WKVb[b_], rBb[b_])

    npair = 4 if dbg >= 4 else 0
    if npair:
        load_pair(0)
    for p in range(npair):
        if p + 1 < npair:
            load_pair(p + 1)
        WQ, WKV, rWp = WQb[p % 2], WKVb[p % 2], rBb[p % 2]
        QP, rQP = a.QP[0], a.rQP[0]
        KP, rKP = a.KP[0], a.rKP[0]
        _proj_feat(k, WQ, rWp, 0, QP, rQP, sc, src=(a.CQN, [a.rCQN], 2))
        _rope_proj(k, a, WQ, rWp, 128, 256, (a.CQN, [a.rCQN], 2), a.QR, a.rQR, sc)
        _proj_feat(k, WKV, rWp, 0, KP, rKP, 1.0, src=(a.CKVN, [a.rCKVN], 1))
        for t in range(NT):
            bi = _nxt(k, "pj")
            bank = 6 + (bi % 2)
            OP(k, "pe", "matmul", [a.rCKVN, rWp], [k.rps[bank]], out=k.ps[bank][:, 0:128],
               lhsT=a.CKVN[:, 0, t * 128:(t + 1) * 128], rhs=WKV[:, 0, 128:256], start=True, stop=True)
            OP(k, "dve", "tensor_copy", [k.rps[bank]], [a.rVP], out=a.VP[:, t, 0, 0:64], in_=k.ps[bank][:, 0:64])
            OP(k, "dve", "tensor_copy", [k.rps[bank]], [a.rVP], out=a.VP[:, t, 1, 64:128], in_=k.ps[bank][:, 64:128])
        _causal_pair(k, a, 4 + p, QP, rQP, KP, rKP, a.VP, a.rVP, None,
                     extra=None if os.environ.get('KNOEXTRA') else (a.QR, a.rQR, a.KR, a.rKR))
    if int(os.environ.get('KDBG', '9')) >= 5:
        _dsa(k, a)
    row_map = {}
    for g in range(4):
        row_map[g] = [(0, 64, 64 * g), (64, 64, 64 * (g + 4))]
        row_map[4 + g] = [(0, 64, 512 + 128 * g), (64, 64, 512 + 128 * g + 64)]
    _wo_apply(k, a, 1, row_map)


def _dsa(k, a):
    wsrc = k.odd_w_in
    KB = 1024
    QC = _view(k, 32 * KB, [128, 4, S], BF16)
    rQC = Res("QC")
    KC2 = _view(k, 78 * KB, [128, S], BF16)
    rKC2 = Res("KC2")
    VP, rVP = a.VP, a.rVP
    QI = _view(k, 66 * KB, [128, 2, S], BF16)
    rQI = Res("QI")
    KI2 = _view(k, 74 * KB, [128, S], BF16)
    rKI2 = Res("KI2")
    WI = _view(k, 82 * KB, [128, NT, 4], F32)
    rWI = Res("WI")
    rb = Res("bis")
    k.P.barrier()
    for g in range(4):
        _wslice(k, wsrc, [(64 * g, 64), (64 * (g + 4), 64)], a.W, a.rW)
        _proj_feat(k, a.W, a.rW, 0, QC[:, g, :], rQC, 0.125)
    kd5 = int(os.environ.get('KD5', '9'))
    if kd5 >= 2:
        _wslice(k, wsrc, [(0, 512, 64), (64, 512, 64), (128, 576, 64), (192, 956, 8), (256, 896, 64), (320, 896, 64)],
                a.W, a.rW)
        _proj_feat(k, a.W, a.rW, 0, KC2, rKC2, 1.0)
        _proj_feat(k, a.W, a.rW, 256, KI2, rKI2, 1.0)
        OP(k, "pool", "memset", [], [rVP], ap=VP, constant=0.0)
    for t in range(NT if kd5 >= 3 else 0):
        bi = _nxt(k, "pj")
        bank = 6 + (bi % 2)
        for c in range(8):
            OP(k, "pe", "matmul", [k.rXT[t], a.rW], [k.rps[bank]], out=k.ps[bank][:, 0:72],
               lhsT=k.XT[:, c, t * 128:(t + 1) * 128], rhs=a.W[:, c, 128:200], start=(c == 0), stop=(c == 7))
        OP(k, "dve", "tensor_copy", [k.rps[bank]], [rVP], out=VP[:, t, 0, 0:64], in_=k.ps[bank][:, 0:64])
        OP(k, "dve", "tensor_copy", [k.rps[bank]], [rVP], out=VP[:, t, 1, 64:128], in_=k.ps[bank][:, 0:64])
        OP(k, "dve", "tensor_scalar", [k.rps[bank]], [rWI], out=WI[:, t, :], in0=k.ps[bank][:, 68:72], scalar1=0.5,
           scalar2=None, op0=ALU.mult)
    for g in range(2 if kd5 >= 4 else 0):
        _wslice(k, wsrc, [(640 + 64 * g, 64), (640 + 64 * (g + 2), 64)], a.W, a.rW)
        _proj_feat(k, a.W, a.rW, 0, QI[:, g, :], rQI, 0.125)
    k.P.barrier()
    dbg = int(os.environ.get('KDBG', '9'))
    if dbg < 6:
        return
    XTf = k.XT[:].rearrange("p c s -> p (c s)")
    MB = XTf[:, 0:2048]
    MBT = XTf[:, 2048:4096].rearrange("p (j t) -> p j t", t=128)
    JK = XTf[:, 4096:6144]
    rSC, rMB, rMBT = Res("SC"), Res("MB"), Res("MBT")

    def xf32(lo_b, n_f32):
        return XTf[:, lo_b // 2:lo_b // 2 + 2 * n_f32].bitcast(F32)
    ABI = xf32(12288, 128).rearrange("p (a h) -> p a h", h=8)
    CMQ = xf32(12800, 128)
    P2 = xf32(13312, 32)
    vec = xf32(13440, 64)
    mid, cnt, tsel, t2, mx, lo = [vec[:, 8 * q:8 * q + 8] for q in range(6)]
    negt = vec[:, 48:49]
    NIT = 26
    WK = xf32(13696, 8 * 32).rearrange("p (b k) -> p b k", k=32)
    SCX = xf32(14848, 4480)
    SCG = k.GB[:].rearrange("p a d -> p (a d)")
    SCW = _view(k, 60 * KB, [128, 1536], F32)
    DMA(k, "sp", [(CMQ, k.c_attn[:, C_CMQ:C_CMQ + 128]),
                  (ABI.rearrange("p a h -> p (a h)"), k.c_attn[:, C_PK:C_PK + 128]),
                  (P2, k.c_attn[:, C_PQ:C_PQ + 32])], [], [rb])
    PKD = [k.xb[0][:].rearrange("p (d s) -> p d s", s=128), k.xb[1][:].rearrange("p (d s) -> p d s", s=128)]
    PQD = _view(k, 83 * KB, [128, 8, 128], BF16)
    DMA(k, "pool", [(k.xb[0][:], k.c_attn[:, C_PKD:C_PKD + 1024]), (k.xb[1][:], k.c_attn[:, C_PKD + 1024:C_PKD + 2048]),
                    (PQD.rearrange("p h t -> p (h t)"), k.c_attn[:, C_PQD:C_PQD + 1024])], [], [rb])
    OP(k, "dve", "memset", [rb], [rb], ap=negt, constant=NEG)
    groups = [[0, 1], [2, 3, 4, 5, 6, 7, 8, 9], [10, 11, 12, 13], [14, 15]]
    place = {0: (SCX, 0), 1: (SCX, 128),
             9: (SCX, 0), 8: (SCX, 1280), 7: (SCX, 2432), 6: (SCX, 3456), 5: (SCG, 0), 4: (SCG, 768), 3: (SCG, 1408),
             2: (SCW, 0),
             13: (SCX, 0), 12: (SCX, 1792), 11: (SCG, 0), 10: (SCW, 0),
             15: (SCX, 0), 14: (SCX, 2048)}
    SCb = {}
    for i_, (buf, o_) in place.items():
        SCb[i_] = buf[:, o_:o_ + (i_ + 1) * 128]

    def indexer(i):
        n = (i + 1) * 128
        SC = SCb[i]
        nkc = (n + 511) // 512
        for kc in range(nkc):
            c0 = kc * 512
            w_ = min(512, n - c0)
            for h in range(4):
                g, hd = h % 2, h // 2
                lb = _nxt(k, "lt") % 4
                OP(k, "pe", "matmul", [rQI, rKI2], [k.rps[lb]], out=k.ps[lb][:, 0:w_],
                   lhsT=QI[64 * hd:64 * hd + 64, g, i * 128:(i + 1) * 128], rhs=KI2[64 * hd:64 * hd + 64, c0:c0 + w_],
                   start=True, stop=True)
                ni = _nxt(k, "nrm") % 2
                tmp, rt_ = a.nrm[ni], a.rnrm[ni]
                OP(k, "act", "activation", [k.rps[lb]], [rt_], out=tmp[:, 0:w_], in_=k.ps[lb][:, 0:w_], func=AF.Relu)
                if h == 0:
                    OP(k, "dve", "tensor_scalar", [rt_, rWI], [rSC], out=SC[:, c0:c0 + w_], in0=tmp[:, 0:w_],
                       scalar1=WI[:, i, 0:1], scalar2=None, op0=ALU.mult)
                else:
                    OP(k, "dve", "scalar_tensor_tensor", [rt_, rWI, rSC], [rSC], out=SC[:, c0:c0 + w_], in0=tmp[:, 0:w_],
                       scalar=WI[:, i, h:h + 1], in1=SC[:, c0:c0 + w_], op0=ALU.mult, op1=ALU.add)
        OP(k, "dve", "tensor_tensor", [rSC, rb], [rSC], out=SC[:, i * 128:n], in0=SC[:, i * 128:n], in1=CMQ, op=ALU.add)

    negmid, sA, nhalf = [xf32(14720, 32)[:, 8 * q:8 * q + 8] for q in range(3)]
    rneg, rsA = Res("negmid"), Res("sA")
    JKA = MBT.rearrange("p j t -> p (j t)")
    colof = {}

    def bisect_group(G):
        nb = len(G)
        na = {8: 3, 4: 2, 2: 1}[nb]
        Dl, Al = G[:nb - na], G[nb - na:]
        for b, i in enumerate(Dl + Al):
            colof[i] = b
        nd = len(Dl)
        for i in G:
            b = colof[i]
            n = (i + 1) * 128
            OP(k, "dve", "tensor_reduce", [rSC], [rb], out=mx[:, b:b + 1], in_=SCb[i][:, 0:n], axis=AX.X, op=ALU.max)
            OP(k, "dve", "tensor_reduce", [rSC], [rb], out=lo[:, b:b + 1], in_=SCb[i][:, 0:i * 128], axis=AX.X, op=ALU.max,
               apply_absolute_value=True, negate=True)
            if i in Al:
                OP(k, "dve", "memset", [rb], [rb], ap=nhalf[:, b:b + 1], constant=0.5 * n)
        OP(k, "dve", "tensor_tensor", [rb], [rb], out=mx[:, 0:nb], in0=mx[:, 0:nb], in1=lo[:, 0:nb], op=ALU.subtract)
        OP(k, "dve", "tensor_tensor", [rb], [rb], out=WK[:, 0:nb, 0:NIT + 1],
           in0=P2[:, 0:NIT + 1].unsqueeze(1).broadcast_to([128, nb, NIT + 1]),
           in1=mx[:, 0:nb].unsqueeze(2).broadcast_to([128, nb, NIT + 1]), op=ALU.mult)
        OP(k, "dve", "tensor_tensor", [rb], [rb], out=mid[:, 0:nb], in0=lo[:, 0:nb], in1=WK[:, 0:nb, 0], op=ALU.add)
        OP(k, "dve", "tensor_scalar", [rb, rsA], [rneg], out=negmid[:, nd:nb], in0=mid[:, nd:nb], scalar1=-1.0, scalar2=None,
           op0=ALU.mult)
        for it in range(NIT):
            for i in Dl:
                b = colof[i]
                n = (i + 1) * 128
                OP(k, "dve", "tensor_scalar", [rSC, rb], [rb], out=JK[:, 0:n], in0=SCb[i][:, 0:n], scalar1=mid[:, b:b + 1],
                   scalar2=None, op0=ALU.is_ge, op1=ALU.add, accum_out=cnt[:, b:b + 1])
            for q, i in enumerate(Al):
                b = colof[i]
                n = (i + 1) * 128
                OP(k, "act", "activation", [rSC, rneg], [rsA] + ([rMBT] if (it == 0 and q == 0) else []), out=JKA[:, 0:n],
                   in_=SCb[i][:, 0:n], func=AF.Sign, bias=negmid[:, b:b + 1], scale=1.0, accum_out=sA[:, b:b + 1])
            OP(k, "dve", "scalar_tensor_tensor", [rsA, rb], [rb], out=cnt[:, nd:nb], in0=sA[:, nd:nb], scalar=0.5,
               in1=nhalf[:, nd:nb], op0=ALU.mult, op1=ALU.add)
            OP(k, "dve", "tensor_scalar", [rb], [rb], out=tsel[:, 0:nb], in0=cnt[:, 0:nb], scalar1=255.5, scalar2=0.5,
               op0=ALU.is_ge, op1=ALU.subtract)
            OP(k, "dve", "tensor_tensor", [rb], [rb], out=t2[:, 0:nb], in0=tsel[:, 0:nb], in1=WK[:, 0:nb, it], op=ALU.mult)
            OP(k, "dve", "tensor_tensor", [rb], [rb], out=mid[:, 0:nb], in0=mid[:, 0:nb], in1=t2[:, 0:nb], op=ALU.add)
            OP(k, "dve", "tensor_scalar", [rb, rsA], [rneg], out=negmid[:, nd:nb], in0=mid[:, nd:nb], scalar1=-1.0,
               scalar2=None, op0=ALU.mult)
        OP(k, "dve", "tensor_tensor", [rb], [rb], out=lo[:, 0:nb], in0=mid[:, 0:nb], in1=WK[:, 0:nb, NIT], op=ALU.subtract)

    def make_mask(i, b):
        n = (i + 1) * 128
        if i >= 2:
            OP(k, "dve", "tensor_scalar", [rSC, rb], [rMB], out=MB[:, 0:n], in0=SCb[i][:, 0:n], scalar1=lo[:, b:b + 1],
               scalar2=negt, op0=ALU.is_lt, op1=ALU.mult)
        else:
            OP(k, "dve", "tensor_scalar", [rSC], [rMB], out=MB[:, 0:n], in0=SCb[i][:, 0:n], scalar1=-5000.0, scalar2=NEG,
               op0=ALU.is_lt, op1=ALU.mult)

    def transposes(i):
        for j0 in range(0, i + 1, 8):
            j1 = min(i + 1, j0 + 8)
            bi = _nxt(k, "pj")
            bank = 6 + (bi % 2)
            pt = k.ps[bank][:].bitcast(BF16)
            for j in range(j0, j1):
                OP(k, "pe", "transpose", [rMB, k.rident], [k.rps[bank]], out=pt[:, (j - j0) * 128:(j - j0 + 1) * 128],
                   in_=MB[:, j * 128:(j + 1) * 128], identity=k.ident[:])
            OP(k, "act", "activation", [k.rps[bank]], [rMBT], out=MBT[:, j0:j1, :],
               in_=pt[:, 0:(j1 - j0) * 128].rearrange("p (j t) -> p j t", t=128), func=AF.Copy)

    def strips(i):
        items = []
        for j in range(i + 1):
            st = {}

            def qk(j=j, st=st):
                ets = []
                for hh in range(2):
                    lb = _nxt(k, "lt") % 4
                    LT = k.ps[lb]
                    LT3 = LT[:].rearrange("p (g t) -> p g t", t=128)
                    OP(k, "pe", "matmul", [rQC, rKC2], [k.rps[lb]], out=LT3,
                       lhsT=KC2[64 * hh:64 * hh + 64, j * 128:(j + 1) * 128],
                       rhs=QC[64 * hh:64 * hh + 64, :, i * 128:(i + 1) * 128], start=True, stop=False)
                    dd = j - i + 15
                    OP(k, "pe", "matmul", [rb], [k.rps[lb]], out=LT3, lhsT=PKD[dd // 8][:, dd % 8, :],
                       rhs=PQD[:, 4 * hh:4 * hh + 4, :], start=False, stop=False)
                    OP(k, "pe", "matmul", [rMBT, k.rident], [k.rps[lb]], out=LT3, lhsT=k.ident[:],
                       rhs=MBT[:, j, :].unsqueeze(1).broadcast_to([128, 4, 128]), start=False, stop=True)
                    ei = _nxt(k, "et") % 4
                    ET, rET = a.ET[ei], a.rET[ei]
                    OP(k, "act", "activation", [k.rps[lb]], [rET], out=ET[:], in_=LT[:], func=AF.Exp)
                    ets.append((ET, rET))
                st["ets"] = ets

            def pv(j=j, st=st):
                for hh in range(2):
                    ET, rET = st["ets"][hh]
                    first = (j == 0 and hh == 0)
                    last = (j == i and hh == 1)
                    OP(k, "pe", "matmul", [rET, rVP], [a.racc], out=k.ps[4][:], lhsT=VP[:, j, hh, :], rhs=ET[:], start=first,
                       stop=last)
                    OP(k, "pe", "matmul", [rET, k.rconst], [a.rden], out=k.ps[5][:], lhsT=k.ONES2[:, hh, :], rhs=ET[:],
                       start=first, stop=last)
            items.append((qk, pv))
        _pipeline(items)

    def normalize(i):
        ni = _nxt(k, "nrm") % 2
        nrm, rn = a.nrm[ni], a.rnrm[ni]
        OP(k, "dve", "reciprocal", [a.rden], [rn], out=nrm[:], in_=k.ps[5][:])
        OP(k, "dve", "tensor_tensor", [a.racc, rn], [a.rattn[g_] for g_ in range(4)], out=a.attnT[:, 0:4, i * 128:(i + 1) * 128],
           in0=k.ps[4][:].rearrange("p (g t) -> p g t", t=128), in1=nrm[:].rearrange("p (g t) -> p g t", t=128), op=ALU.mult)

    for G in groups:
        for i in G:
            indexer(i)
        if G[0] >= 2 and not os.environ.get('DSA_NOBIS'):
            bisect_group(G)
        for b, i in enumerate(G):
            make_mask(i, colof.get(i, b))
            transposes(i)
            if not os.environ.get('DSA_NOSTRIPS'):
                strips(i)
                normalize(i)


def _consts():
    c = np.zeros((128, CONST_COLS), np.float32)
    sr = np.arange(128)[:, None]
    tr = np.arange(128)[None, :]
    c[:, C_CM:C_CM + 128] = np.where(sr <= tr, 0.0, NEG)
    slopes = 2.0 ** (-8.0 * np.arange(1, 9) / 8.0)
    ma = np.zeros((128, 8, 2, 128), np.float32)
    for h in range(8):
        d0 = (tr - sr).astype(np.float32)
        ma[:, h, 0, :] = np.where(sr <= tr, -slopes[h] * d0, NEG)
        d1 = (tr - sr + 128).astype(np.float32)
        ma[:, h, 1, :] = np.where(sr > tr, -slopes[h] * d1, NEG)
    c[:, C_MA:C_MA + 2048] = ma.reshape(128, 2048)
    c[:, C_TRI:C_TRI + 128] = (sr <= tr).astype(np.float32)
    c[:, C_CMQ:C_CMQ + 128] = np.where(tr <= sr, 0.0, -1.0e4)
    pos = np.arange(S, dtype=np.float64)
    inv = 10000.0 ** (-np.arange(16, dtype=np.float64) / 16.0)
    ang = pos[None, :] * inv[:, None]
    for r in range(128):
        d = r % 32
        c[r, C_ROPE:C_ROPE + S] = np.cos(ang[d % 16])
        sg = -1.0 if d < 16 else 1.0
        c[r, C_ROPE + S:C_ROPE + 2 * S] = sg * np.sin(ang[d % 16])
    c[:, C_PQ:C_PQ + 32] = (2.0 ** -(np.arange(32) + 1.0))[None, :]
    pkd = np.zeros((128, 16, 128), np.float32)
    for dd in range(16):
        pkd[0, dd, :] = 128.0 * (dd - 15)
        pkd[1, dd, :] = np.arange(128)
        pkd[2, dd, :] = 1.0
    pqd = np.zeros((128, 8, 128), np.float32)
    for h in range(8):
        pqd[0, h, :] = slopes[h]
        pqd[1, h, :] = slopes[h]
        pqd[2, h, :] = -slopes[h] * np.arange(128)
    c[:, C_PKD:C_PKD + 2048] = pkd.reshape(128, 2048)
    c[:, C_PQD:C_PQD + 1024] = pqd.reshape(128, 1024)
    abi = np.zeros((128, 16, 8), np.float32)
    for dd in range(16):
        for h in range(8):
            abi[:, dd, h] = slopes[h] * (128.0 * (dd - 15) + np.arange(128))
    c[:, C_PK:C_PK + 128] = abi.reshape(128, 128)
    return c
    pk = np.zeros((4, 16, 128), np.float32)
    for dd in range(16):
        pk[0, dd, :] = 128.0 * (dd - 15)
        pk[1, dd, :] = np.arange(128)
        pk[2, dd, :] = 1.0
    pq = np.zeros((4, 8, 128), np.float32)
    for h in range(8):
        pq[0, h, :] = slopes[h]
        pq[1, h, :] = slopes[h]
        pq[2, h, :] = -slopes[h] * np.arange(128)
    c[0:4, C_PK:C_PK + 2048] = pk.reshape(4, 2048)
    c[0:4, C_PQ:C_PQ + 1024] = pq.reshape(4, 1024)
    return c


def kernel(**inputs):
    n = 8
    nseq = 32 // n
    nc = build(nseq)
    shared = {}
    for name in ("even_w_in", "even_b_f", "even_sinks", "odd_w_in", "odd_q_norm", "odd_kv_norm", "odd_w_uq",
                 "odd_w_ukv"):
        shared[name] = np.ascontiguousarray(np.asarray(inputs[name], np.float32)[0])
    for name in ("w_o", "ln_g", "ln_b", "router_w", "router_b", "moe_w_gate", "moe_w_up", "moe_w_down"):
        shared[name] = np.ascontiguousarray(np.asarray(inputs[name], np.float32))
    shared["c_attn"] = _consts()
    x = np.asarray(inputs["x"], np.float32)
    in_maps = []
    for c in range(n):
        m = dict(shared)
        m["x"] = np.ascontiguousarray(x[c * nseq:(c + 1) * nseq])
        in_maps.append(m)
    res = run_bass_kernel_spmd(nc, in_maps, core_ids=list(range(n)))
    return np.concatenate([r["out"] for r in res.results], axis=0)
```

```python
import contextlib
import os
import numpy as np
import concourse.bass as bass
import concourse.mybir as mybir
from concourse.bass_utils import run_bass_kernel_spmd

F32 = mybir.dt.float32
BF16 = mybir.dt.bfloat16
AF = mybir.ActivationFunctionType
ALU = mybir.AluOpType
AX = mybir.AxisListType

ENGS = ("pe", "act", "dve", "pool", "sp")

D = 1024
S = 2048
NT = 16
NE = 16
DFF = 512
ALPHA = 4.0 ** 0.25
LN_EPS = 1e-5
RMS_EPS = 1e-6
NEG = -30000.0


class Res:
    __slots__ = ("name", "w", "r")

    def __init__(self, name):
        self.name = name
        self.w = None
        self.r = []


class Op:
    __slots__ = ("eng", "idx", "fn", "deps", "is_dma", "ndma", "sem", "cnt", "needed", "_order", "attach")
    _ctr = [0]

    def __init__(self, eng, idx, fn, is_dma, ndma):
        self.eng = eng
        self.idx = idx
        self.fn = fn
        self.is_dma = is_dma
        self.ndma = ndma
        self.deps = []
        self.sem = None
        self.cnt = 0
        self.needed = False
        self.attach = False
        Op._ctr[0] += 1
        self._order = Op._ctr[0]


class Prog:
    NDMASEM = 24

    def __init__(self, nc):
        self.nc = nc
        self.ops = {e: [] for e in ENGS}
        self.waited = {e: {f: -1 for f in ENGS} for e in ENGS}
        self.dma_rr = 0
        self.dma_last = [None] * self.NDMASEM
        self.dma_waited = {e: set() for e in ENGS}
        self.all_dma = []

    def op(self, eng, fn, reads=(), writes=(), dma=0, selfdep=True):
        o = Op(eng, len(self.ops[eng]), fn, dma > 0, dma)
        deps = []
        for R in reads:
            if R.w is not None:
                deps.append(R.w)
        for R in writes:
            if R.w is not None:
                deps.append(R.w)
            deps.extend(R.r)
        if o.is_dma:
            half = self.NDMASEM // 2
            key = "rr_" + eng
            i_ = getattr(self, key, 0)
            setattr(self, key, i_ + 1)
            slot = (i_ % half) + (half if eng == "pool" else 0)
            prev = self.dma_last[slot]
            if prev is not None:
                deps.append(prev)
            self.dma_last[slot] = o
            o.sem = slot
            self.all_dma.append(o)
        best = {}
        for d in deps:
            if d is o:
                continue
            if d.is_dma:
                if id(d) in self.dma_waited[eng]:
                    continue
                best[("dma", id(d))] = d
            else:
                if d.eng == eng and (eng == "pe" or not selfdep):
                    continue
                if d.idx <= self.waited[eng][d.eng]:
                    continue
                k = ("e", d.eng)
                if k not in best or best[k].idx < d.idx:
                    best[k] = d
        for k, d in best.items():
            d.needed = True
            o.deps.append(d)
            if d.is_dma:
                self.dma_waited[eng].add(id(d))
            else:
                self.waited[eng][d.eng] = d.idx
        for R in reads:
            R.r.append(o)
        for R in writes:
            R.w = o
            R.r = []
        self.ops[eng].append(o)
        return o

    def barrier(self):
        lasts = []
        for e in ENGS:
            if e == "sp":
                continue
            for o_ in reversed(self.ops[e]):
                if o_.fn is not None and not o_.is_dma:
                    lasts.append(o_)
                    break
        dmas = list(self.all_dma)
        for e in ENGS:
            o = Op(e, len(self.ops[e]), None, False, 0)
            for d in lasts:
                if d.eng == e or d.is_dma:
                    continue
                if d.idx <= self.waited[e][d.eng]:
                    continue
                d.needed = True
                o.deps.append(d)
                self.waited[e][d.eng] = d.idx
            for d in dmas:
                if id(d) in self.dma_waited[e]:
                    continue
                d.needed = True
                o.deps.append(d)
                self.dma_waited[e].add(id(d))
            self.ops[e].append(o)
        self.all_dma = []

    def emit(self):
        nc = self.nc
        with contextlib.ExitStack() as st:
            esem = {e: st.enter_context(nc.semaphore("s_" + e)) for e in ENGS}
            dsem = [st.enter_context(nc.semaphore("d%d" % i)) for i in range(self.NDMASEM)]
            for e in ENGS:
                c = 0
                for o in self.ops[e]:
                    if o.is_dma:
                        continue
                    if o.needed:
                        c += 1
                        o.cnt = c
            dc = [0] * self.NDMASEM
            alld = []
            for e in ENGS:
                for o in self.ops[e]:
                    if o.is_dma:
                        alld.append(o)
            alld.sort(key=lambda o: o._order)
            for o in alld:
                dc[o.sem] += 16 * o.ndma
                o.cnt = dc[o.sem]
            block = st.enter_context(nc.Block())

            def run(eng_name, eng):
                for o in self.ops[eng_name]:
                    deps = list(o.deps)
                    emb = None
                    if o.fn is not None and o.attach and deps:
                        emb = deps.pop(0)
                    for d in deps:
                        if d.is_dma:
                            eng.wait_ge(dsem[d.sem], d.cnt)
                        else:
                            eng.wait_ge(esem[d.eng], d.cnt)
                    if o.fn is None:
                        continue
                    r = o.fn(eng)
                    if emb is not None:
                        r0 = r[0] if isinstance(r, (list, tuple)) else r
                        if emb.is_dma:
                            r0._wait_ge(dsem[emb.sem], emb.cnt)
                        else:
                            r0._wait_ge(esem[emb.eng], emb.cnt)
                    if o.is_dma:
                        if not isinstance(r, (list, tuple)):
                            r = [r]
                        assert len(r) == o.ndma, (len(r), o.ndma)
                        for ins in r:
                            ins.then_inc(dsem[o.sem], 16)
                    elif o.needed:
                        if isinstance(r, (list, tuple)):
                            r = r[-1]
                        r.then_inc(esem[eng_name], 1)

            @block.tensor
            def _(eng):
                run("pe", eng)

            @block.scalar
            def _(eng):
                run("act", eng)

            @block.vector
            def _(eng):
                run("dve", eng)

            @block.gpsimd
            def _(eng):
                run("pool", eng)

            @block.sync
            def _(eng):
                run("sp", eng)


class K:
    pass


def _rr(lst, state, key):
    i = state.get(key, 0)
    state[key] = i + 1
    return lst[i % len(lst)]


def build(nseq, do_attn=True, do_moe=True, layers=(0, 1)):
    nc = bass.Bass("TRN2", target_bir_lowering=False)
    k = K()
    k.nc = nc
    k.nseq = nseq
    dt = nc.dram_tensor
    k.x = dt("x", [nseq, S, D], F32, kind="ExternalInput").ap()
    k.out = dt("out", [nseq, S, D], F32, kind="ExternalOutput").ap()
    k.even_w_in = dt("even_w_in", [D, 2312], F32, kind="ExternalInput").ap()
    k.even_b_f = dt("even_b_f", [8], F32, kind="ExternalInput").ap()
    k.even_sinks = dt("even_sinks", [8], F32, kind="ExternalInput").ap()
    k.odd_w_in = dt("odd_w_in", [D, 1380], F32, kind="ExternalInput").ap()
    k.odd_q_norm = dt("odd_q_norm", [256], F32, kind="ExternalInput").ap()
    k.odd_kv_norm = dt("odd_kv_norm", [128], F32, kind="ExternalInput").ap()
    k.odd_w_uq = dt("odd_w_uq", [256, 768], F32, kind="ExternalInput").ap()
    k.odd_w_ukv = dt("odd_w_ukv", [128, 1024], F32, kind="ExternalInput").ap()
    k.w_o = dt("w_o", [2, 1024, 1024], F32, kind="ExternalInput").ap()
    k.ln_g = dt("ln_g", [2, 2, D], F32, kind="ExternalInput").ap()
    k.ln_b = dt("ln_b", [2, 2, D], F32, kind="ExternalInput").ap()
    k.router_w = dt("router_w", [D, NE], F32, kind="ExternalInput").ap()
    k.router_b = dt("router_b", [NE], F32, kind="ExternalInput").ap()
    k.moe_w_gate = dt("moe_w_gate", [2, NE, D, DFF], F32, kind="ExternalInput").ap()
    k.moe_w_up = dt("moe_w_up", [2, NE, D, DFF], F32, kind="ExternalInput").ap()
    k.moe_w_down = dt("moe_w_down", [2, NE, DFF, D], F32, kind="ExternalInput").ap()
    k.c_attn = dt("c_attn", [128, CONST_COLS], F32, kind="ExternalInput").ap()

    with contextlib.ExitStack() as st:
        k.st = st
        k.P = Prog(nc)
        k.rr = {}
        _alloc_common(k)
        for s in range(nseq):
            _load_seq(k, s)
            for l in layers:
                if do_attn:
                    if l == 0:
                        _mixer_even(k)
                    else:
                        _mixer_odd(k)
                    if do_moe:
                        _moe_prefetch(k, l)
                    _layernorm(k, l, 0, last=(not do_moe and l == layers[-1]))
                if do_moe:
                    _moe(k, l)
                    _layernorm(k, l, 1, last=(l == layers[-1]))
            _store_seq(k, s)
        k.P.barrier()
        k.P.emit()
    return nc


C_CM = 0
C_MA = 128
C_TRI = 128 + 2048
C_CMQ = C_TRI + 128
C_ROPE = C_CMQ + 128
C_PK = C_ROPE + 4096
C_PQ = C_PK + 2048
C_PKD = C_PQ + 1024
C_PQD = C_PKD + 2048
CONST_COLS = C_PQD + 1024
SH_COLS = 22016


def _view(k, off_bytes, shape, dtype):
    n = 1
    for d_ in shape[1:]:
        n *= d_
    c0 = off_bytes // 4
    if dtype == BF16:
        ap = k.SH[:, c0:c0 + n // 2].bitcast(BF16)
    else:
        ap = k.SH[:, c0:c0 + n]
    if len(shape) == 2:
        return ap
    names = " ".join("d%d" % i for i in range(1, len(shape)))
    kw = {"d%d" % i: shape[i] for i in range(1, len(shape) - 1)}
    return ap.rearrange("p (%s) -> p %s" % (names, names), **kw)


def _T(k, name, shape, dtype):
    return k.st.enter_context(k.nc.sbuf_tensor(name, shape, dtype))


def _alloc_common(k):
    nc, P = k.nc, k.P
    k.X = _T(k, "X", [128, NT, D], F32)
    k.XT = _T(k, "XT", [128, 8, S], BF16)
    k.rX = [Res("X%d" % t) for t in range(NT)]
    k.rXT = [Res("XT%d" % t) for t in range(NT)]
    k.ident = _T(k, "ident", [128, 128], BF16)
    k.rident = Res("ident")
    k.eps = _T(k, "eps", [128, 2], F32)
    k.GB = _T(k, "GB", [128, 2, D], F32)
    k.rGB = Res("GB")
    k.xb = [_T(k, "xb%d" % i, [128, D], BF16) for i in range(2)]
    k.rxb = [Res("xb%d" % i) for i in range(2)]
    k.lnst = [_T(k, "lnst%d" % i, [128, 16], F32) for i in range(2)]
    k.rlnst = [Res("lnst%d" % i) for i in range(2)]
    k.ps = [k.st.enter_context(nc.psum_tensor("ps%d" % i, [128, 512], F32)) for i in range(8)]
    k.rps = [Res("ps%d" % i) for i in range(8)]
    P.op("pool", lambda e: e.memset(k.ident[:], 0.0), writes=[k.rident])
    P.op("pool", lambda e: e.affine_select(out=k.ident[:], in_=k.ident[:], pattern=[[-1, 128]],
                                          compare_op=ALU.not_equal, fill=1.0, base=0,
                                          channel_multiplier=1),
         reads=[k.rident], writes=[k.rident])
    P.op("pool", lambda e: e.memset(k.eps[:, 0:1], LN_EPS), writes=[k.rident])
    P.op("pool", lambda e: e.memset(k.eps[:, 1:2], RMS_EPS), writes=[k.rident])
    k.SH = _T(k, "SH", [128, SH_COLS], F32)
    k.wg = [_view(k, (24 * i) * 1024, [128, 8, DFF], BF16) for i in range(2)]
    k.wu = [_view(k, (24 * i + 8) * 1024, [128, 8, DFF], BF16) for i in range(2)]
    k.wd = [_view(k, (24 * i + 16) * 1024, [128, 4, D], BF16) for i in range(2)]
    k.rw = [Res("w%d" % i) for i in range(2)]
    k.hT = [_view(k, (48 + 4 * i) * 1024, [128, 4, 512], BF16) for i in range(2)]
    k.rhT = [Res("hT%d" % i) for i in range(2)]
    k.sg = [_view(k, (56 + 2 * i) * 1024, [128, 512], F32) for i in range(2)]
    k.rsg = [Res("sg%d" % i) for i in range(2)]
    k.rt = [_view(k, (60 + 2 * i) * 1024, [128, 512], F32) for i in range(6)]
    k.rrt = Res("rt")
    k.CMb = _T(k, "CMb", [128, 128], BF16)
    k.MAb = _T(k, "MAb", [128, 8, 2, 128], BF16)
    k.TRI = _T(k, "TRI", [128, 128], F32)
    k.ONESF = _T(k, "ONESF", [128, 128], F32)
    k.ONES2 = _T(k, "ONES2", [128, 2, 128], BF16)
    k.one1 = _T(k, "one1", [128, 1], F32)
    k.esink = _T(k, "esink", [128, 4], F32)
    k.bfb = _T(k, "bfb", [128, 8], F32)
    k.rconst = Res("const")
    k.ONESb = _T(k, "ONESb", [128, 128], BF16)
    k.gq = _T(k, "gq", [128, 2], F32)
    k.gkv = _T(k, "gkv", [128, 1], F32)
    k.nrm0 = _T(k, "nrm0", [128, 512], F32)
    k.nrm1 = _T(k, "nrm1", [128, 512], F32)
    DMA(k, "pool", [(k.CMb[:], k.c_attn[:, C_CM:C_CM + 128]),
                    (k.MAb[:].rearrange("p h r t -> p (h r t)"), k.c_attn[:, C_MA:C_MA + 2048])], [], [k.rconst])
    DMA(k, "sp", [(k.TRI[:], k.c_attn[:, C_TRI:C_TRI + 128]),
                  (k.esink[0:64, :], k.even_sinks[0:4].partition_broadcast(64)),
                  (k.esink[64:128, :], k.even_sinks[4:8].partition_broadcast(64)),
                  (k.bfb[:], k.even_b_f.partition_broadcast(128))], [], [k.rconst])
    OP(k, "pool", "memset", [], [k.rconst], ap=k.ONESF[:], constant=1.0)
    OP(k, "pool", "memset", [], [k.rconst], ap=k.ONESb[:], constant=1.0)
    DMA(k, "sp", [(k.gq[:, 0:1], k.odd_q_norm[0:128].rearrange("(p o) -> p o", o=1)),
                  (k.gq[:, 1:2], k.odd_q_norm[128:256].rearrange("(p o) -> p o", o=1)),
                  (k.gkv[:], k.odd_kv_norm.rearrange("(p o) -> p o", o=1))], [], [k.rconst])
    OP(k, "pool", "memset", [], [k.rconst], ap=k.one1[:], constant=1.0)
    OP(k, "pool", "memset", [], [k.rconst], ap=k.ONES2[:], constant=0.0)
    OP(k, "pool", "memset", [k.rconst], [k.rconst], ap=k.ONES2[:, 0, 0:64], constant=1.0)
    OP(k, "pool", "memset", [k.rconst], [k.rconst], ap=k.ONES2[:, 1, 64:128], constant=1.0)
    OP(k, "act", "activation", [k.rconst], [k.rconst], out=k.esink[:], in_=k.esink[:], func=AF.Exp)
    k.rwt = _T(k, "rwt", [128, 8, NE], BF16)
    k.rrwt = Res("rwt")
    k.rbt = _T(k, "rbt", [128, NE], F32)
    k.gates = _T(k, "gates", [128, NT * NE], F32)
    k.rgates = Res("gates")
    P.op("pool", lambda e: e.dma_start(out=k.rwt[:], in_=k.router_w.rearrange("(c p) e -> p c e", p=128)),
         writes=[k.rrwt], dma=1)
    P.op("sp", lambda e: e.dma_start(out=k.rbt[:], in_=k.router_b.partition_broadcast(128)),
         writes=[k.rrwt], dma=1)


_ATTACH_OK = {"matmul", "transpose", "activation", "tensor_tensor", "tensor_copy", "tensor_scalar", "scalar_tensor_tensor",
              "reciprocal", "tensor_reduce"}


def OP(k, eng, name, reads, writes, **kw):
    o = k.P.op(eng, lambda e, kw=kw: getattr(e, name)(**kw), reads=reads, writes=writes)
    ok = name in _ATTACH_OK and kw.get("accum_out") is None and os.environ.get("KNOATTACH") is None
    if ok and name == "matmul" and kw["lhsT"].dtype == F32:
        ok = False
    o.attach = ok
    return o


def DMA(k, eng, pairs, reads, writes):
    return k.P.op(eng, lambda e, pairs=pairs: [e.dma_start(out=o, in_=i) for (o, i) in pairs],
                  reads=reads, writes=writes, dma=len(pairs))


def _nxt(k, key):
    i = k.rr.get(key, 0)
    k.rr[key] = i + 1
    return i


def _transpose_tile(k, t, scale):
    i = _nxt(k, "xb")
    xb, rxb = k.xb[i % 2], k.rxb[i % 2]
    OP(k, "act", "activation", [k.rX[t]], [rxb], out=xb[:], in_=k.X[:, t, :], func=AF.Copy, scale=scale)
    bank = 6 + (i % 2)
    pt = k.ps[bank][:].bitcast(BF16)
    for c in range(8):
        OP(k, "pe", "transpose", [rxb, k.rident], [k.rps[bank]], out=pt[:, c * 128:(c + 1) * 128],
           in_=xb[:, c * 128:(c + 1) * 128], identity=k.ident[:])
    OP(k, "dve", "tensor_copy", [k.rps[bank]], [k.rXT[t]], out=k.XT[:, :, t * 128:(t + 1) * 128],
       in_=pt.rearrange("p (c t) -> p c t", c=8))


def _load_seq(k, s):
    for t in range(NT):
        DMA(k, "sp", [(k.X[:, t, :], k.x[s, t * 128:(t + 1) * 128, :])], [], [k.rX[t]])
    for t in range(NT):
        _transpose_tile(k, t, 1.0)
        OP(k, "pool", "tensor_scalar", [k.rX[t]], [k.rX[t]], out=k.X[:, t, :], in0=k.X[:, t, :], scalar1=ALPHA,
           scalar2=None, op0=ALU.mult)


def _store_seq(k, s):
    for t in range(NT):
        DMA(k, "sp", [(k.out[s, t * 128:(t + 1) * 128, :], k.X[:, t, :])], [k.rX[t]], [])


def _layernorm(k, l, j, last):
    DMA(k, "sp", [(k.GB[:, 0, :], k.ln_g[l, j].partition_broadcast(128)),
                  (k.GB[:, 1, :], k.ln_b[l, j].partition_broadcast(128))], [], [k.rGB])
    if not last:
        OP(k, "pool", "tensor_scalar", [k.rGB], [k.rGB], out=k.GB[:], in0=k.GB[:], scalar1=ALPHA, scalar2=None,
           op0=ALU.mult)
    for t in range(NT):
        i = _nxt(k, "ln")
        stt, rst = k.lnst[i % 2], k.rlnst[i % 2]
        Xt = k.X[:, t, :]
        rX = k.rX[t]
        for h in range(2):
            OP(k, "dve", "bn_stats", [rX], [rst], out=stt[:, h * 6:(h + 1) * 6], in_=Xt[:, h * 512:(h + 1) * 512])
        OP(k, "dve", "bn_aggr", [rst], [rst], out=stt[:, 12:14], in_=stt[:, 0:12].rearrange("p (a b) -> p a b", a=2))
        OP(k, "act", "activation", [rst, k.rident], [rst], out=stt[:, 14:15], in_=stt[:, 13:14], func=AF.Sqrt,
           bias=k.eps[:, 0:1], scale=1.0)
        OP(k, "dve", "reciprocal", [rst], [rst], out=stt[:, 14:15], in_=stt[:, 14:15])
        OP(k, "dve", "scalar_tensor_tensor", [rst], [rst], out=stt[:, 15:16], in0=stt[:, 12:13], scalar=-1.0,
           in1=stt[:, 14:15], op0=ALU.mult, op1=ALU.mult)
        OP(k, "act", "activation", [rX, rst], [rX], out=Xt, in_=Xt, func=AF.Identity, bias=stt[:, 15:16],
           scale=stt[:, 14:15])
        OP(k, "dve", "tensor_tensor", [rX, k.rGB], [rX], out=Xt, in0=Xt, in1=k.GB[:, 0, :], op=ALU.mult)
        OP(k, "pool", "tensor_tensor", [rX, k.rGB], [rX], out=Xt, in0=Xt, in1=k.GB[:, 1, :], op=ALU.add)
        if not last:
            _transpose_tile(k, t, 1.0 / ALPHA)


def _load_expert(k, l, e, slot):
    DMA(k, "pool", [
        (k.wg[slot][:], k.moe_w_gate[l, e].rearrange("(c p) f -> p c f", p=128)),
        (k.wu[slot][:], k.moe_w_up[l, e].rearrange("(c p) f -> p c f", p=128)),
        (k.wd[slot][:], k.moe_w_down[l, e].rearrange("(c p) f -> p c f", p=128)),
    ], [], [k.rw[slot]])


def _router(k):
    bank = 5
    pr = k.ps[bank]
    for t in range(NT):
        for c in range(8):
            OP(k, "pe", "matmul", [k.rXT[t], k.rrwt], [k.rps[bank]], out=pr[:, t * 16:(t + 1) * 16],
               lhsT=k.XT[:, c, t * 128:(t + 1) * 128], rhs=k.rwt[:, c, :], start=(c == 0), stop=(c == 7))
    aff, sel, pairs, t3, t4, t5 = [x for x in k.rt]
    R = [k.rrt]
    N = NT * NE
    G = NT * 4
    OP(k, "act", "activation", [k.rps[bank]], R, out=aff[:, 0:N], in_=pr[:, 0:N], func=AF.Sigmoid)
    OP(k, "dve", "tensor_tensor", R + [k.rrwt], R, out=sel[:, 0:N].rearrange("p (t e) -> p t e", e=NE),
       in0=aff[:, 0:N].rearrange("p (t e) -> p t e", e=NE),
       in1=k.rbt[:].unsqueeze(1).broadcast_to([128, NT, NE]), op=ALU.add)
    sel4 = sel[:, 0:N].rearrange("p (g e) -> p g e", e=4)
    pv = pairs[:, 0:G * 6].rearrange("p (g q) -> p g q", q=6)
    pi = 0
    for a in range(4):
        for b in range(a + 1, 4):
            OP(k, "dve", "tensor_tensor", R, R, out=pv[:, :, pi:pi + 1], in0=sel4[:, :, a:a + 1],
               in1=sel4[:, :, b:b + 1], op=ALU.add)
            pi += 1
    gs = t3[:, 0:G]
    m1 = t3[:, G:2 * G]
    thr2 = t3[:, 2 * G:3 * G]
    gmax = t3[:, 3 * G:3 * G + NT]
    gmask = t3[:, 4 * G:5 * G]
    OP(k, "dve", "tensor_reduce", R, R, out=gs, in_=pv, axis=AX.X, op=ALU.max)
    OP(k, "dve", "tensor_reduce", R, R, out=m1, in_=sel4, axis=AX.X, op=ALU.max)
    em1 = t4[:, 0:N].rearrange("p (g e) -> p g e", e=4)
    selm = t5[:, 0:N].rearrange("p (g e) -> p g e", e=4)
    OP(k, "dve", "tensor_tensor", R, R, out=em1, in0=sel4, in1=m1.unsqueeze(2).broadcast_to([128, G, 4]),
       op=ALU.is_ge)
    OP(k, "dve", "scalar_tensor_tensor", R, R, out=selm, in0=em1, scalar=-1.0e9, in1=sel4, op0=ALU.mult, op1=ALU.add)
    OP(k, "dve", "tensor_reduce", R, R, out=thr2, in_=selm, axis=AX.X, op=ALU.max)
    OP(k, "dve", "tensor_reduce", R, R, out=gmax, in_=gs.rearrange("p (t g) -> p t g", g=4), axis=AX.X, op=ALU.max)
    OP(k, "dve", "tensor_tensor", R, R, out=gmask.rearrange("p (t g) -> p t g", g=4),
       in0=gs.rearrange("p (t g) -> p t g", g=4), in1=gmax.unsqueeze(2).broadcast_to([128, NT, 4]), op=ALU.is_ge)
    em = t4[:, 0:N].rearrange("p (g e) -> p g e", e=4)
    OP(k, "dve", "tensor_tensor", R, R, out=em, in0=sel4, in1=thr2.unsqueeze(2).broadcast_to([128, G, 4]),
       op=ALU.is_ge)
    OP(k, "dve", "tensor_tensor", R, R, out=em, in0=em, in1=gmask.unsqueeze(2).broadcast_to([128, G, 4]),
       op=ALU.mult)
    ga = t5[:, 0:N]
    OP(k, "dve", "tensor_tensor", R, R, out=ga, in0=t4[:, 0:N], in1=aff[:, 0:N], op=ALU.mult)
    den = t5[:, N:N + NT]
    OP(k, "dve", "tensor_reduce", R, R, out=den, in_=ga.rearrange("p (t e) -> p t e", e=NE), axis=AX.X, op=ALU.add)
    OP(k, "dve", "reciprocal", R, R, out=den, in_=den)
    OP(k, "dve", "tensor_tensor", R, [k.rgates], out=k.gates[:].rearrange("p (t e) -> p t e", e=NE),
       in0=ga.rearrange("p (t e) -> p t e", e=NE), in1=den.unsqueeze(2).broadcast_to([128, NT, NE]), op=ALU.mult)


def _moe_prefetch(k, l):
    _load_expert(k, l, 0, 0)
    _load_expert(k, l, 1, 1)
    k.moe_pref = True


def _moe(k, l):
    _router(k)
    if not getattr(k, "moe_pref", False):
        _moe_prefetch(k, l)
    k.moe_pref = False
    items = []
    for e in range(NE):
        slot = e % 2
        wg, wu, wd, rw = k.wg[slot], k.wu[slot], k.wd[slot], k.rw[slot]
        for tc in range(4):
            st = {}

            def gu(e=e, tc=tc, st=st, wg=wg, wu=wu, rw=rw):
                hi = _nxt(k, "hT")
                hT, rhT = k.hT[hi % 2], k.rhT[hi % 2]
                st["hT"], st["rhT"] = hT, rhT
                cols = slice(tc * 512, (tc + 1) * 512)
                rxt = [k.rXT[tc * 4 + i] for i in range(4)]
                for ff in range(4):
                    gi = _nxt(k, "gu")
                    bg, bu = (gi % 2) * 2, (gi % 2) * 2 + 1
                    for c in range(8):
                        OP(k, "pe", "matmul", rxt + [rw], [k.rps[bg]], out=k.ps[bg][:],
                           lhsT=wg[:, c, ff * 128:(ff + 1) * 128], rhs=k.XT[:, c, cols], start=(c == 0), stop=(c == 7))
                    for c in range(8):
                        OP(k, "pe", "matmul", rxt + [rw], [k.rps[bu]], out=k.ps[bu][:],
                           lhsT=wu[:, c, ff * 128:(ff + 1) * 128], rhs=k.XT[:, c, cols], start=(c == 0), stop=(c == 7))
                    sg, rsg = k.sg[gi % 2], k.rsg[gi % 2]
                    OP(k, "act", "activation", [k.rps[bg]], [rsg], out=sg[:], in_=k.ps[bg][:], func=AF.Silu)
                    OP(k, "dve", "tensor_tensor", [rsg, k.rps[bu]], [rhT], out=hT[:, ff, :], in0=sg[:], in1=k.ps[bu][:],
                       op=ALU.mult)

            def down(e=e, tc=tc, st=st, wd=wd, rw=rw):
                hT, rhT = st["hT"], st["rhT"]
                for tt in range(4):
                    t = tc * 4 + tt
                    for half in range(2):
                        yi = _nxt(k, "y")
                        by = 4 + (yi % 4)
                        for ff in range(4):
                            OP(k, "pe", "matmul", [rhT, rw], [k.rps[by]], out=k.ps[by][:],
                               lhsT=hT[:, ff, tt * 128:(tt + 1) * 128], rhs=wd[:, ff, half * 512:(half + 1) * 512],
                               start=(ff == 0), stop=(ff == 3))
                        Xs = k.X[:, t, half * 512:(half + 1) * 512]
                        OP(k, "dve", "scalar_tensor_tensor", [k.rps[by], k.rgates, k.rX[t]], [k.rX[t]], out=Xs,
                           in0=k.ps[by][:], scalar=k.gates[:, t * NE + e:t * NE + e + 1], in1=Xs, op0=ALU.mult,
                           op1=ALU.add)
                if tc == 3 and e + 2 < NE:
                    _load_expert(k, l, e + 2, e % 2)
            items.append((gu, down))
    _pipeline(items)


def _proj_feat(k, W, rW, wc0, dst, rdst, scale, nrow=128, src=None, evac=None):
    for tcn in range(4):
        bi = _nxt(k, "pj")
        bank = 6 + (bi % 2)
        if src is None:
            sap, rs, nch = k.XT, [k.rXT[tcn * 4 + i] for i in range(4)], 8
        else:
            sap, rs, nch = src
        for c in range(nch):
            OP(k, "pe", "matmul", rs + [rW], [k.rps[bank]], out=k.ps[bank][0:nrow, :],
               lhsT=W[:, c, wc0:wc0 + nrow], rhs=sap[:, c, tcn * 512:(tcn + 1) * 512], start=(c == 0), stop=(c == nch - 1))
        if evac is not None:
            evac(tcn, bank)
        else:
            OP(k, "act", "activation", [k.rps[bank]], [rdst], out=dst[0:nrow, tcn * 512:(tcn + 1) * 512],
               in_=k.ps[bank][0:nrow, :], func=AF.Copy, scale=scale)


def _proj_tok(k, W, rW, wc0, ncols, t, outs, routs):
    bi = _nxt(k, "pj")
    bank = 6 + (bi % 2)
    for c in range(8):
        OP(k, "pe", "matmul", [k.rXT[t], rW], [k.rps[bank]], out=k.ps[bank][:, 0:ncols],
           lhsT=k.XT[:, c, t * 128:(t + 1) * 128], rhs=W[:, c, wc0:wc0 + ncols], start=(c == 0), stop=(c == 7))
    for (dst, lo, hi) in outs:
        OP(k, "dve", "tensor_copy", [k.rps[bank]], routs, out=dst, in_=k.ps[bank][:, lo:hi])


def _wslice(k, wsrc, col_ranges, W, rW):
    pairs = []
    off = 0
    v = wsrc.rearrange("(c p) f -> p c f", p=128)
    for cr in col_ranges:
        if len(cr) == 3:
            off, c0, n = cr
        else:
            c0, n = cr
        pairs.append((W[:, :, off:off + n], v[:, :, c0:c0 + n]))
        off += n
    DMA(k, "pool", pairs, [], [rW])


def _attn_views(k):
    a = K()
    a.attnT = _view(k, 0, [128, 8, S], BF16)
    a.rattn = [Res("attn%d" % c) for c in range(8)]
    a.QP = [_view(k, (32 + 4 * i) * 1024, [128, S], BF16) for i in range(2)]
    a.rQP = [Res("QP%d" % i) for i in range(2)]
    a.KP = [_view(k, (40 + 4 * i) * 1024, [128, S], BF16) for i in range(2)]
    a.rKP = [Res("KP%d" % i) for i in range(2)]
    a.VP = _view(k, 48 * 1024, [128, NT, 2, 128], BF16)
    a.rVP = Res("VP")
    a.ET = [_view(k, (56 + i) * 1024, [128, 512], BF16) for i in range(4)]
    a.rET = [Res("ET%d" % i) for i in range(4)]
    a.W = _view(k, 60 * 1024, [128, 8, 384], BF16)
    a.rW = Res("Wsl")
    a.FB = _view(k, 66 * 1024, [128, 4, NT, 8], F32)
    a.cp = _view(k, 68 * 1024, [128, NT, 8], F32)
    a.Tp = _view(k, 68 * 1024 + 512, [128, NT + 1, 8], F32)
    a.TT = _view(k, 69 * 1024 + 128, [128, NT, 8], F32)
    a.zt = _view(k, 70 * 1024, [128, NT, 8], F32)
    a.nrm = [_view(k, 32 * 1024 + 0, [128, 512], F32)]
    a.nrm = [k.nrm0, k.nrm1]
    a.rnrm = [Res("nrm0"), Res("nrm1")]
    a.rfox = Res("fox")
    a.WO = _view(k, 32 * 1024, [128, 8, D], BF16)
    a.rWO = Res("WO")
    a.racc = Res("acc")
    a.rden = Res("den")
    return a


def _wo_apply(k, a, l, row_map):
    k.P.barrier()
    pairs = []
    for c in range(8):
        for (plo, n, r0) in row_map[c]:
            pairs.append((a.WO[plo:plo + n, c, :], k.w_o[l, r0:r0 + n, :]))
    DMA(k, "pool", pairs, [], [a.rWO])
    for t in range(NT):
        for half in range(2):
            bi = _nxt(k, "pj")
            bank = 6 + (bi % 2)
            for c in range(8):
                OP(k, "pe", "matmul", [a.rattn[c], a.rWO], [k.rps[bank]], out=k.ps[bank][:],
                   lhsT=a.attnT[:, c, t * 128:(t + 1) * 128], rhs=a.WO[:, c, half * 512:(half + 1) * 512],
                   start=(c == 0), stop=(c == 7))
            Xs = k.X[:, t, half * 512:(half + 1) * 512]
            OP(k, "dve", "tensor_tensor", [k.rps[bank], k.rX[t]], [k.rX[t]], out=Xs, in0=k.ps[bank][:], in1=Xs,
               op=ALU.add)
    k.P.barrier()


def _pipeline(items):
    if not items:
        return
    items[0][0]()
    for n_ in range(len(items)):
        if n_ + 1 < len(items):
            items[n_ + 1][0]()
        items[n_][1]()


def _causal_pair(k, a, chunk, QP, rQP, KP, rKP, VP, rVP, bias_fn, krows=64, extra=None):
    items = []
    for c in range(4):
        nj = 4 * c + 4
        for j in range(nj):
            st = {}

            def qk(c=c, j=j, st=st):
                a0 = max(0, j - 4 * c) * 128
                diag = j >= 4 * c
                ets = []
                for hd in range(2):
                    lb = _nxt(k, "lt") % 4
                    LT = k.ps[lb]
                    kk = KP[64 * hd:64 * hd + krows, j * 128:(j + 1) * 128]
                    qbase = c * 512
                    if extra is not None:
                        QR, rQR, KR, rKR = extra
                        OP(k, "pe", "matmul", [rQR, rKR], [k.rps[lb]], out=LT[:, a0:512],
                           lhsT=KR[64 * hd:64 * hd + 64, j * 128:(j + 1) * 128],
                           rhs=QR[64 * hd:64 * hd + 64, qbase + a0:qbase + 512], start=True, stop=False)
                    st0 = extra is None
                    if diag:
                        OP(k, "pe", "matmul", [rQP, rKP], [k.rps[lb]], out=LT[:, a0:a0 + 128], lhsT=kk,
                           rhs=QP[64 * hd:64 * hd + krows, qbase + a0:qbase + a0 + 128], start=st0, stop=False)
                        if st0:
                            OP(k, "pe", "matmul", [k.rconst, k.rident], [k.rps[lb]], out=LT[:, a0:a0 + 128], lhsT=k.ident[:],
                               rhs=k.CMb[:], start=False, stop=True)
                        if a0 + 128 < 512:
                            OP(k, "pe", "matmul", [rQP, rKP], [k.rps[lb]], out=LT[:, a0 + 128:512], lhsT=kk,
                               rhs=QP[64 * hd:64 * hd + krows, qbase + a0 + 128:qbase + 512], start=st0, stop=st0)
                        if not st0:
                            OP(k, "pe", "matmul", [k.rconst, k.rident], [k.rps[lb]], out=LT[:, a0:a0 + 128], lhsT=k.ident[:],
                               rhs=k.CMb[:], start=False, stop=True)
                    else:
                        OP(k, "pe", "matmul", [rQP, rKP], [k.rps[lb]], out=LT[:, 0:512], lhsT=kk,
                           rhs=QP[64 * hd:64 * hd + krows, qbase:qbase + 512], start=st0, stop=True)
                    ei = _nxt(k, "et") % 4
                    ET, rET = a.ET[ei], a.rET[ei]
                    b = bias_fn(c, j, hd) if bias_fn is not None else None
                    if b is not None:
                        OP(k, "act", "activation", [k.rps[lb], a.rfox], [rET], out=ET[:, a0:512], in_=LT[:, a0:512],
                           func=AF.Exp, bias=b, scale=1.0)
                    else:
                        OP(k, "act", "activation", [k.rps[lb]], [rET], out=ET[:, a0:512], in_=LT[:, a0:512], func=AF.Exp)
                    ets.append((ET, rET))
                st["ets"] = ets
                st["a0"] = a0

            def pv(c=c, j=j, nj=nj, st=st):
                a0 = st["a0"]
                for hd in range(2):
                    ET, rET = st["ets"][hd]
                    first = (j == 0 and hd == 0)
                    last = (j == nj - 1 and hd == 1)
                    OP(k, "pe", "matmul", [rET, rVP], [a.racc], out=k.ps[4][:, a0:512], lhsT=VP[:, j, hd, :],
                       rhs=ET[:, a0:512], start=first, stop=last)
                    OP(k, "pe", "matmul", [rET, k.rconst], [a.rden], out=k.ps[5][:, a0:512], lhsT=k.ONES2[:, hd, :],
                       rhs=ET[:, a0:512], start=first, stop=last)
                if j == nj - 1:
                    ni = _nxt(k, "nrm") % 2
                    nrm, rn = a.nrm[ni], a.rnrm[ni]
                    OP(k, "dve", "reciprocal", [a.rden], [rn], out=nrm[:], in_=k.ps[5][:])
                    OP(k, "dve", "tensor_tensor", [a.racc, rn], [a.rattn[chunk]],
                       out=a.attnT[:, chunk, c * 512:(c + 1) * 512], in0=k.ps[4][:], in1=nrm[:], op=ALU.mult)
            items.append((qk, pv))
    _pipeline(items)


def _mixer_even(k):
    k.P.barrier()
    a = _attn_views(k)
    wsrc = k.even_w_in
    Wb = [a.W, _view(k, 72 * 1024, [128, 8, 384], BF16)]
    rWb = [a.rW, Res("Wsl2")]
    specs = [[(2304, 8)], [(512, 128), (640, 128)]]
    specs += [[(64 * p_, 64), (64 * (p_ + 4), 64)] for p_ in range(4)]
    specs += [[(768 + 128 * p_, 128), (1280 + 128 * p_, 128), (1792 + 128 * p_, 128)] for p_ in range(4)]
    wst = {"n": 0}

    def take_w():
        n_ = wst["n"]
        if n_ == 0:
            _wslice(k, wsrc, specs[0], Wb[0], rWb[0])
        if n_ + 1 < len(specs):
            _wslice(k, wsrc, specs[n_ + 1], Wb[(n_ + 1) % 2], rWb[(n_ + 1) % 2])
        wst["n"] = n_ + 1
        return Wb[n_ % 2], rWb[n_ % 2]
    Wc, rWc = take_w()
    bank = 6
    for t in range(NT):
        for c in range(8):
            OP(k, "pe", "matmul", [k.rXT[t], rWc], [k.rps[bank]], out=k.ps[bank][:, t * 8:(t + 1) * 8],
               lhsT=k.XT[:, c, t * 128:(t + 1) * 128], rhs=Wc[:, c, 0:8], start=(c == 0), stop=(c == 7))
    R = [a.rfox]
    OP(k, "dve", "tensor_tensor", [k.rps[bank], k.rconst], R, out=a.zt, in0=k.ps[bank][:, 0:128].rearrange("p (t h) -> p t h", h=8),
       in1=k.bfb[:].unsqueeze(1).broadcast_to([128, NT, 8]), op=ALU.add)
    OP(k, "act", "activation", R, R, out=a.zt, in_=a.zt, func=AF.Exp, scale=-1.0)
    OP(k, "act", "activation", R + [k.rconst], R, out=a.zt, in_=a.zt, func=AF.Ln, bias=k.one1[:], scale=1.0)
    ztf = a.zt.rearrange("p t h -> p (t h)")
    b2 = 7
    OP(k, "pe", "matmul", R + [k.rconst], [k.rps[b2]], out=k.ps[b2][:, 0:128], lhsT=k.TRI[:], rhs=ztf, start=True, stop=True)
    OP(k, "pe", "matmul", R + [k.rconst], [k.rps[b2]], out=k.ps[b2][:, 128:256], lhsT=k.ONESF[:], rhs=ztf, start=True, stop=True)
    OP(k, "dve", "tensor_copy", [k.rps[b2]], R, out=a.TT, in_=k.ps[b2][:, 128:256].rearrange("p (t h) -> p t h", h=8))
    OP(k, "dve", "memset", [], R, ap=a.Tp[:, 0, :], constant=0.0)
    for j in range(NT):
        OP(k, "dve", "tensor_tensor", R, R, out=a.Tp[:, j + 1, :], in0=a.Tp[:, j, :], in1=a.TT[:, j, :], op=ALU.add)
    OP(k, "dve", "tensor_tensor", [k.rps[b2]] + R, R, out=a.cp, in0=k.ps[b2][:, 0:128].rearrange("p (t h) -> p t h", h=8),
       in1=a.Tp[:, 0:NT, :], op=ALU.add)
    for c in range(4):
        OP(k, "dve", "tensor_tensor", R, R, out=a.FB[:, c, :, :], in0=a.cp,
           in1=a.Tp[:, 4 * c, :].unsqueeze(1).broadcast_to([128, NT, 8]), op=ALU.subtract)
    dbg = int(os.environ.get('KDBG', '9'))
    OP(k, "pool", "memset", [], [a.rVP], ap=a.VP, constant=0.0)
    Wc, rWc = take_w()
    K2, rK2 = a.KP[0], a.rKP[0]
    _proj_feat(k, Wc, rWc, 0, K2, rK2, 1.0)
    for t in range(NT):
        _proj_tok(k, Wc, rWc, 128, 128, t, [(a.VP[:, t, 0, 0:64], 0, 64), (a.VP[:, t, 1, 64:128], 64, 128)], [a.rVP])
    for p in range(4 if dbg >= 2 else 0):
        Wc, rWc = take_w()
        QP, rQP = a.QP[p % 2], a.rQP[p % 2]
        _proj_feat(k, Wc, rWc, 0, QP, rQP, 0.125)
        items = []
        for i in range(NT):
            st = {}

            def qk(i=i, st=st, p=p, QP=QP, rQP=rQP):
                lb = _nxt(k, "lt") % 4
                LT = k.ps[lb]
                jl = [i] if i == 0 else [i - 1, i]
                idx = 0
                for jj in jl:
                    rel = i - jj
                    for hd in range(2):
                        h = p + 4 * hd
                        o = LT[:, idx * 128:(idx + 1) * 128]
                        OP(k, "pe", "matmul", [rQP, rK2], [k.rps[lb]], out=o, lhsT=K2[64 * hd:64 * hd + 64, jj * 128:(jj + 1) * 128],
                           rhs=QP[64 * hd:64 * hd + 64, i * 128:(i + 1) * 128], start=True, stop=False)
                        OP(k, "pe", "matmul", [k.rconst, k.rident], [k.rps[lb]], out=o, lhsT=k.ident[:], rhs=k.MAb[:, h, rel, :],
                           start=False, stop=True)
                        idx += 1
                n = idx * 128
                ei = _nxt(k, "et") % 4
                ET, rET = a.ET[ei], a.rET[ei]
                OP(k, "act", "activation", [k.rps[lb]], [rET], out=ET[:, 0:n], in_=LT[:, 0:n], func=AF.Exp)
                st["ET"], st["rET"], st["jl"] = ET, rET, jl

            def pv(i=i, st=st, p=p):
                ET, rET, jl = st["ET"], st["rET"], st["jl"]
                sl = (i % 4) * 128
                idx = 0
                for jj in jl:
                    for hd in range(2):
                        first, last = (idx == 0), (idx == 2 * len(jl) - 1)
                        OP(k, "pe", "matmul", [rET, a.rVP], [a.racc], out=k.ps[4][:, 0:128], lhsT=a.VP[:, jj, hd, :],
                           rhs=ET[:, idx * 128:(idx + 1) * 128], start=first, stop=last)
                        OP(k, "pe", "matmul", [rET, k.rconst], [a.rden], out=k.ps[5][:, 0:128], lhsT=k.ONES2[:, hd, :],
                           rhs=ET[:, idx * 128:(idx + 1) * 128], start=first, stop=last)
                        idx += 1
                ni = _nxt(k, "nrm") % 2
                nrm, rn = a.nrm[ni], a.rnrm[ni]
                OP(k, "dve", "tensor_scalar", [a.rden, k.rconst], [rn], out=nrm[:, 0:128], in0=k.ps[5][:, 0:128],
                   scalar1=k.esink[:, p:p + 1], scalar2=None, op0=ALU.add)
                OP(k, "dve", "reciprocal", [rn], [rn], out=nrm[:, 0:128], in_=nrm[:, 0:128])
                OP(k, "dve", "tensor_tensor", [a.racc, rn], [a.rattn[p]], out=a.attnT[:, p, i * 128:(i + 1) * 128],
                   in0=k.ps[4][:, 0:128], in1=nrm[:, 0:128], op=ALU.mult)
            items.append((qk, pv))
        _pipeline(items)
    for p in range(4 if dbg >= 3 else 0):
        Wc, rWc = take_w()
        QP, rQP = a.QP[p % 2], a.rQP[p % 2]
        KP, rKP = a.KP[p % 2], a.rKP[p % 2]
        _proj_feat(k, Wc, rWc, 0, QP, rQP, 0.125)
        _proj_feat(k, Wc, rWc, 128, KP, rKP, 1.0)
        for t in range(NT):
            _proj_tok(k, Wc, rWc, 256, 128, t, [(a.VP[:, t, 0, 0:64], 0, 64), (a.VP[:, t, 1, 64:128], 64, 128)], [a.rVP])
        _causal_pair(k, a, 4 + p, QP, rQP, KP, rKP, a.VP, a.rVP,
                     lambda c, j, hd, p=p: a.FB[:, c, j, 2 * p + hd:2 * p + hd + 1])
    row_map = {}
    for p in range(4):
        row_map[p] = [(0, 64, 64 * p), (64, 64, 64 * (p + 4))]
        row_map[4 + p] = [(0, 128, 512 + 128 * p)]
    _wo_apply(k, a, 0, row_map)


def _rmsnorm_feat(k, a, src, rsrc, nch, gt, dst, rdst, RS, rRS):
    for tcn in range(4):
        cols = slice(tcn * 512, (tcn + 1) * 512)
        bi = _nxt(k, "pj")
        bank = 6 + (bi % 2)
        for c in range(nch):
            ei = _nxt(k, "et") % 4
            OP(k, "dve", "tensor_tensor", rsrc, [a.rET[ei]], out=a.ET[ei][:], in0=src[:, c, cols], in1=src[:, c, cols],
               op=ALU.mult)
            OP(k, "pe", "matmul", [a.rET[ei], k.rconst], [k.rps[bank]], out=k.ps[bank][:], lhsT=k.ONESb[:], rhs=a.ET[ei][:],
               start=(c == 0), stop=(c == nch - 1))
        OP(k, "act", "activation", [k.rps[bank], k.rident], [rRS], out=RS[:, cols], in_=k.ps[bank][:], func=AF.Sqrt,
           bias=k.eps[:, 1:2], scale=1.0 / (128.0 * nch))
        OP(k, "dve", "reciprocal", [rRS], [rRS], out=RS[:, cols], in_=RS[:, cols])
        for c in range(nch):
            OP(k, "dve", "scalar_tensor_tensor", rsrc + [rRS, k.rconst], [rdst], out=dst[:, c, cols], in0=src[:, c, cols],
               scalar=gt[:, c:c + 1], in1=RS[:, cols], op0=ALU.mult, op1=ALU.mult)


def _rope_evac(k, a, bankA, bankB, dst, rdst, cols, scale):
    t1, r1 = a.nrm[0], a.rnrm[0]
    t2, r2 = a.nrm[1], a.rnrm[1]
    OP(k, "dve", "scalar_tensor_tensor", [k.rps[bankA], a.rrope], [r1], out=t1[:, :], in0=k.ps[bankA][:, :], scalar=scale,
       in1=a.ROPE[:, 0, cols], op0=ALU.mult, op1=ALU.mult)
    OP(k, "dve", "scalar_tensor_tensor", [k.rps[bankB], a.rrope], [r2], out=t2[:, :], in0=k.ps[bankB][:, :], scalar=scale,
       in1=a.ROPE[:, 1, cols], op0=ALU.mult, op1=ALU.mult)
    OP(k, "pool", "tensor_tensor", [r1, r2], [rdst], out=dst[:, cols], in0=t1[:, :], in1=t2[:, :], op=ALU.add)


def _rope_proj(k, a, W, rW, cA, cB, src, dst, rdst, scale):
    sap, rs, nch = src
    for tcn in range(4):
        cols = slice(tcn * 512, (tcn + 1) * 512)
        rr_ = rs if rs is not None else [k.rXT[tcn * 4 + i] for i in range(4)]
        for (bank, c0) in ((6, cA), (7, cB)):
            for c in range(nch):
                OP(k, "pe", "matmul", rr_ + [rW], [k.rps[bank]], out=k.ps[bank][:, :], lhsT=W[:, c, c0:c0 + 128],
                   rhs=sap[:, c, cols], start=(c == 0), stop=(c == nch - 1))
        _rope_evac(k, a, 6, 7, dst, rdst, cols, scale)


def _mixer_odd(k):
    k.P.barrier()
    a = _attn_views(k)
    wsrc = k.odd_w_in
    KB = 1024
    a.CQN = _view(k, 66 * KB, [128, 2, S], BF16)
    a.rCQN = Res("CQN")
    a.CKVN = _view(k, 74 * KB, [128, 1, S], BF16)
    a.rCKVN = Res("CKVN")
    a.ROPE = _view(k, 78 * KB, [128, 2, S], BF16)
    a.rrope = Res("rope")
    a.QR = a.QP[1]
    a.rQR = a.rQP[1]
    a.KR = a.KP[1]
    a.rKR = a.rKP[1]
    RS = a.VP.rearrange("p t v d -> p (t v d)").bitcast(F32)
    gq = k.gq
    DMA(k, "pool", [(a.ROPE.rearrange("p a s -> p (a s)"), k.c_attn[:, C_ROPE:C_ROPE + 4096])], [], [a.rrope])
    raw = _view(k, 32 * KB, [128, 2, S], BF16)
    rraw = a.rQP[0]
    _wslice(k, wsrc, [(964, 256), (1220, 128)], a.W, a.rW)
    _proj_feat(k, a.W, a.rW, 0, raw[:, 0, :], a.rQP[0], 1.0)
    _proj_feat(k, a.W, a.rW, 128, raw[:, 1, :], a.rQP[1], 1.0)
    rawkv = _view(k, 40 * KB, [128, 1, S], BF16)
    _proj_feat(k, a.W, a.rW, 256, rawkv[:, 0, :], a.rKP[0], 1.0)
    class _RR:
        pass
    for tcn in range(4):
        pass
    dbg = int(os.environ.get('KDBG', '9'))
    if dbg >= 2:
        _rmsnorm_feat(k, a, raw, [a.rQP[0], a.rQP[1]], 2, k.gq, a.CQN, a.rCQN, RS, a.rVP)
        _rmsnorm_feat(k, a, rawkv, [a.rKP[0]], 1, k.gkv, a.CKVN, a.rCKVN, RS, a.rVP)
    OP(k, "pool", "memset", [a.rW], [a.rW], ap=a.W[:, :, 0:256], constant=0.0)
    _wslice(k, wsrc, [(0, 1348, 32), (64, 1348, 32), (128, 1364, 16), (144, 1348, 16), (192, 1364, 16), (208, 1348, 16)],
            a.W, a.rW)
    if dbg >= 3:
        _rope_proj(k, a, a.W, a.rW, 0, 128, (k.XT, None, 8), a.KR, a.rKR, 1.0)
    OP(k, "pool", "memset", [a.rVP], [a.rVP], ap=a.VP, constant=0.0)
    WQb = [_view(k, 60 * KB, [128, 2, 384], BF16), _view(k, 62 * KB, [128, 2, 384], BF16)]
    WKVb = [_view(k, 61 * KB + 512, [128, 1, 256], BF16), _view(k, 63 * KB + 512, [128, 1, 256], BF16)]
    rBb = [Res("WB0"), Res("WB1")]
    sc = 96.0 ** -0.5

    def load_pair(p):
        b_ = p % 2
        h0, h1 = 2 * p, 2 * p + 1
        extra = [a.rW] if p < 2 else []
        OP(k, "pool", "memset", [], [rBb[b_]] + extra, ap=WQb[b_][:, :, 128:384], constant=0.0)
        _wslice(k, k.odd_w_uq, [(0, h0 * 96, 64), (64, h1 * 96, 64), (128, h0 * 96 + 64, 32), (192, h1 * 96 + 64, 32),
                                (256, h0 * 96 + 80, 16), (272, h0 * 96 + 64, 16), (320, h1 * 96 + 80, 16),
                                (336, h1 * 96 + 64, 16)], WQb[b_], rBb[b_])
        _wslice(k, k.odd_w_ukv, [(h0 * 128, 64), (h1 * 128, 64), (h0 * 128 + 64, 64), (h1 * 128 + 64, 64)], WKVb[b_], rBb[b_])

    npair = 4 if dbg >= 4 else 0
    if npair:
        load_pair(0)
    for p in range(npair):
        if p + 1 < npair:
            load_pair(p + 1)
        WQ, WKV, rWp = WQb[p % 2], WKVb[p % 2], rBb[p % 2]
        QP, rQP = a.QP[0], a.rQP[0]
        KP, rKP = a.KP[0], a.rKP[0]
        _proj_feat(k, WQ, rWp, 0, QP, rQP, sc, src=(a.CQN, [a.rCQN], 2))
        _rope_proj(k, a, WQ, rWp, 128, 256, (a.CQN, [a.rCQN], 2), a.QR, a.rQR, sc)
        _proj_feat(k, WKV, rWp, 0, KP, rKP, 1.0, src=(a.CKVN, [a.rCKVN], 1))
        for t in range(NT):
            bi = _nxt(k, "pj")
            bank = 6 + (bi % 2)
            OP(k, "pe", "matmul", [a.rCKVN, rWp], [k.rps[bank]], out=k.ps[bank][:, 0:128],
               lhsT=a.CKVN[:, 0, t * 128:(t + 1) * 128], rhs=WKV[:, 0, 128:256], start=True, stop=True)
            OP(k, "dve", "tensor_copy", [k.rps[bank]], [a.rVP], out=a.VP[:, t, 0, 0:64], in_=k.ps[bank][:, 0:64])
            OP(k, "dve", "tensor_copy", [k.rps[bank]], [a.rVP], out=a.VP[:, t, 1, 64:128], in_=k.ps[bank][:, 64:128])
        _causal_pair(k, a, 4 + p, QP, rQP, KP, rKP, a.VP, a.rVP, None,
                     extra=None if os.environ.get('KNOEXTRA') else (a.QR, a.rQR, a.KR, a.rKR))
    if int(os.environ.get('KDBG', '9')) >= 5:
        _dsa(k, a)
    row_map = {}
    for g in range(4):
        row_map[g] = [(0, 64, 64 * g), (64, 64, 64 * (g + 4))]
        row_map[4 + g] = [(0, 64, 512 + 128 * g), (64, 64, 512 + 128 * g + 64)]
    _wo_apply(k, a, 1, row_map)


def _dsa(k, a):
    wsrc = k.odd_w_in
    KB = 1024
    QC = _view(k, 32 * KB, [128, 4, S], BF16)
    rQC = Res("QC")
    KC2 = _view(k, 78 * KB, [128, S], BF16)
    rKC2 = Res("KC2")
    VP, rVP = a.VP, a.rVP
    QI = _view(k, 66 * KB, [128, 2, S], BF16)
    rQI = Res("QI")
    KI2 = _view(k, 74 * KB, [128, S], BF16)
    rKI2 = Res("KI2")
    WI = _view(k, 82 * KB, [128, NT, 4], F32)
    rWI = Res("WI")
    rb = Res("bis")
    k.P.barrier()
    for g in range(4):
        _wslice(k, wsrc, [(64 * g, 64), (64 * (g + 4), 64)], a.W, a.rW)
        _proj_feat(k, a.W, a.rW, 0, QC[:, g, :], rQC, 0.125)
    kd5 = int(os.environ.get('KD5', '9'))
    if kd5 >= 2:
        _wslice(k, wsrc, [(0, 512, 64), (64, 512, 64), (128, 576, 64), (192, 956, 8), (256, 896, 64), (320, 896, 64)],
                a.W, a.rW)
        _proj_feat(k, a.W, a.rW, 0, KC2, rKC2, 1.0)
        _proj_feat(k, a.W, a.rW, 256, KI2, rKI2, 1.0)
        OP(k, "pool", "memset", [], [rVP], ap=VP, constant=0.0)
    for t in range(NT if kd5 >= 3 else 0):
        bi = _nxt(k, "pj")
        bank = 6 + (bi % 2)
        for c in range(8):
            OP(k, "pe", "matmul", [k.rXT[t], a.rW], [k.rps[bank]], out=k.ps[bank][:, 0:72],
               lhsT=k.XT[:, c, t * 128:(t + 1) * 128], rhs=a.W[:, c, 128:200], start=(c == 0), stop=(c == 7))
        OP(k, "dve", "tensor_copy", [k.rps[bank]], [rVP], out=VP[:, t, 0, 0:64], in_=k.ps[bank][:, 0:64])
        OP(k, "dve", "tensor_copy", [k.rps[bank]], [rVP], out=VP[:, t, 1, 64:128], in_=k.ps[bank][:, 0:64])
        OP(k, "dve", "tensor_scalar", [k.rps[bank]], [rWI], out=WI[:, t, :], in0=k.ps[bank][:, 68:72], scalar1=0.5,
           scalar2=None, op0=ALU.mult)
    for g in range(2 if kd5 >= 4 else 0):
        _wslice(k, wsrc, [(640 + 64 * g, 64), (640 + 64 * (g + 2), 64)], a.W, a.rW)
        _proj_feat(k, a.W, a.rW, 0, QI[:, g, :], rQI, 0.125)
    k.P.barrier()
    dbg = int(os.environ.get('KDBG', '9'))
    if dbg < 6:
        return
    XTf = k.XT[:].rearrange("p c s -> p (c s)")
    MB = XTf[:, 0:2048]
    MBT = XTf[:, 2048:4096].rearrange("p (j t) -> p j t", t=128)
    JK = XTf[:, 4096:6144]
    rSC, rMB, rMBT = Res("SC"), Res("MB"), Res("MBT")

    def xf32(lo_b, n_f32):
        return XTf[:, lo_b // 2:lo_b // 2 + 2 * n_f32].bitcast(F32)
    ABI = xf32(12288, 128).rearrange("p (a h) -> p a h", h=8)
    CMQ = xf32(12800, 128)
    P2 = xf32(13312, 32)
    vec = xf32(13440, 64)
    mid, cnt, tsel, t2, mx, lo = [vec[:, 8 * q:8 * q + 8] for q in range(6)]
    negt = vec[:, 48:49]
    NIT = 26
    WK = xf32(13696, 8 * 32).rearrange("p (b k) -> p b k", k=32)
    SCX = xf32(14848, 4480)
    SCG = k.GB[:].rearrange("p a d -> p (a d)")
    SCW = _view(k, 60 * KB, [128, 1536], F32)
    DMA(k, "sp", [(CMQ, k.c_attn[:, C_CMQ:C_CMQ + 128]),
                  (ABI.rearrange("p a h -> p (a h)"), k.c_attn[:, C_PK:C_PK + 128]),
                  (P2, k.c_attn[:, C_PQ:C_PQ + 32])], [], [rb])
    PKD = [k.xb[0][:].rearrange("p (d s) -> p d s", s=128), k.xb[1][:].rearrange("p (d s) -> p d s", s=128)]
    PQD = _view(k, 83 * KB, [128, 8, 128], BF16)
    DMA(k, "pool", [(k.xb[0][:], k.c_attn[:, C_PKD:C_PKD + 1024]), (k.xb[1][:], k.c_attn[:, C_PKD + 1024:C_PKD + 2048]),
                    (PQD.rearrange("p h t -> p (h t)"), k.c_attn[:, C_PQD:C_PQD + 1024])], [], [rb])
    OP(k, "dve", "memset", [rb], [rb], ap=negt, constant=NEG)
    groups = [[0, 1], [2, 3, 4, 5, 6, 7, 8, 9], [10, 11, 12, 13], [14, 15]]
    place = {0: (SCX, 0), 1: (SCX, 128),
             9: (SCX, 0), 8: (SCX, 1280), 7: (SCX, 2432), 6: (SCX, 3456), 5: (SCG, 0), 4: (SCG, 768), 3: (SCG, 1408),
             2: (SCW, 0),
             13: (SCX, 0), 12: (SCX, 1792), 11: (SCG, 0), 10: (SCW, 0),
             15: (SCX, 0), 14: (SCX, 2048)}
    SCb = {}
    for i_, (buf, o_) in place.items():
        SCb[i_] = buf[:, o_:o_ + (i_ + 1) * 128]

    def indexer(i):
        n = (i + 1) * 128
        SC = SCb[i]
        nkc = (n + 511) // 512
        for kc in range(nkc):
            c0 = kc * 512
            w_ = min(512, n - c0)
            for h in range(4):
                g, hd = h % 2, h // 2
                lb = _nxt(k, "lt") % 4
                OP(k, "pe", "matmul", [rQI, rKI2], [k.rps[lb]], out=k.ps[lb][:, 0:w_],
                   lhsT=QI[64 * hd:64 * hd + 64, g, i * 128:(i + 1) * 128], rhs=KI2[64 * hd:64 * hd + 64, c0:c0 + w_],
                   start=True, stop=True)
                ni = _nxt(k, "nrm") % 2
                tmp, rt_ = a.nrm[ni], a.rnrm[ni]
                OP(k, "act", "activation", [k.rps[lb]], [rt_], out=tmp[:, 0:w_], in_=k.ps[lb][:, 0:w_], func=AF.Relu)
                if h == 0:
                    OP(k, "dve", "tensor_scalar", [rt_, rWI], [rSC], out=SC[:, c0:c0 + w_], in0=tmp[:, 0:w_],
                       scalar1=WI[:, i, 0:1], scalar2=None, op0=ALU.mult)
                else:
                    OP(k, "dve", "scalar_tensor_tensor", [rt_, rWI, rSC], [rSC], out=SC[:, c0:c0 + w_], in0=tmp[:, 0:w_],
                       scalar=WI[:, i, h:h + 1], in1=SC[:, c0:c0 + w_], op0=ALU.mult, op1=ALU.add)
        OP(k, "dve", "tensor_tensor", [rSC, rb], [rSC], out=SC[:, i * 128:n], in0=SC[:, i * 128:n], in1=CMQ, op=ALU.add)

    negmid, sA, nhalf = [xf32(14720, 32)[:, 8 * q:8 * q + 8] for q in range(3)]
    rneg, rsA = Res("negmid"), Res("sA")
    JKA = MBT.rearrange("p j t -> p (j t)")
    colof = {}

    def bisect_group(G):
        nb = len(G)
        na = {8: 3, 4: 2, 2: 1}[nb]
        Dl, Al = G[:nb - na], G[nb - na:]
        for b, i in enumerate(Dl + Al):
            colof[i] = b
        nd = len(Dl)
        for i in G:
            b = colof[i]
            n = (i + 1) * 128
            OP(k, "dve", "tensor_reduce", [rSC], [rb], out=mx[:, b:b + 1], in_=SCb[i][:, 0:n], axis=AX.X, op=ALU.max)
            OP(k, "dve", "tensor_reduce", [rSC], [rb], out=lo[:, b:b + 1], in_=SCb[i][:, 0:i * 128], axis=AX.X, op=ALU.max,
               apply_absolute_value=True, negate=True)
            if i in Al:
                OP(k, "dve", "memset", [rb], [rb], ap=nhalf[:, b:b + 1], constant=0.5 * n)
        OP(k, "dve", "tensor_tensor", [rb], [rb], out=mx[:, 0:nb], in0=mx[:, 0:nb], in1=lo[:, 0:nb], op=ALU.subtract)
        OP(k, "dve", "tensor_tensor", [rb], [rb], out=WK[:, 0:nb, 0:NIT + 1],
           in0=P2[:, 0:NIT + 1].unsqueeze(1).broadcast_to([128, nb, NIT + 1]),
           in1=mx[:, 0:nb].unsqueeze(2).broadcast_to([128, nb, NIT + 1]), op=ALU.mult)
        OP(k, "dve", "tensor_tensor", [rb], [rb], out=mid[:, 0:nb], in0=lo[:, 0:nb], in1=WK[:, 0:nb, 0], op=ALU.add)
        OP(k, "dve", "tensor_scalar", [rb, rsA], [rneg], out=negmid[:, nd:nb], in0=mid[:, nd:nb], scalar1=-1.0, scalar2=None,
           op0=ALU.mult)
        for it in range(NIT):
            for i in Dl:
                b = colof[i]
                n = (i + 1) * 128
                OP(k, "dve", "tensor_scalar", [rSC, rb], [rb], out=JK[:, 0:n], in0=SCb[i][:, 0:n], scalar1=mid[:, b:b + 1],
                   scalar2=None, op0=ALU.is_ge, op1=ALU.add, accum_out=cnt[:, b:b + 1])
            for q, i in enumerate(Al):
                b = colof[i]
                n = (i + 1) * 128
                OP(k, "act", "activation", [rSC, rneg], [rsA] + ([rMBT] if (it == 0 and q == 0) else []), out=JKA[:, 0:n],
                   in_=SCb[i][:, 0:n], func=AF.Sign, bias=negmid[:, b:b + 1], scale=1.0, accum_out=sA[:, b:b + 1])
            OP(k, "dve", "scalar_tensor_tensor", [rsA, rb], [rb], out=cnt[:, nd:nb], in0=sA[:, nd:nb], scalar=0.5,
               in1=nhalf[:, nd:nb], op0=ALU.mult, op1=ALU.add)
            OP(k, "dve", "tensor_scalar", [rb], [rb], out=tsel[:, 0:nb], in0=cnt[:, 0:nb], scalar1=255.5, scalar2=0.5,
               op0=ALU.is_ge, op1=ALU.subtract)
            OP(k, "dve", "tensor_tensor", [rb], [rb], out=t2[:, 0:nb], in0=tsel[:, 0:nb], in1=WK[:, 0:nb, it], op=ALU.mult)
            OP(k, "dve", "tensor_tensor", [rb], [rb], out=mid[:, 0:nb], in0=mid[:, 0:nb], in1=t2[:, 0:nb], op=ALU.add)
            OP(k, "dve", "tensor_scalar", [rb, rsA], [rneg], out=negmid[:, nd:nb], in0=mid[:, nd:nb], scalar1=-1.0,
               scalar2=None, op0=ALU.mult)
        OP(k, "dve", "tensor_tensor", [rb], [rb], out=lo[:, 0:nb], in0=mid[:, 0:nb], in1=WK[:, 0:nb, NIT], op=ALU.subtract)

    def make_mask(i, b):
        n = (i + 1) * 128
        if i >= 2:
            OP(k, "dve", "tensor_scalar", [rSC, rb], [rMB], out=MB[:, 0:n], in0=SCb[i][:, 0:n], scalar1=lo[:, b:b + 1],
               scalar2=negt, op0=ALU.is_lt, op1=ALU.mult)
        else:
            OP(k, "dve", "tensor_scalar", [rSC], [rMB], out=MB[:, 0:n], in0=SCb[i][:, 0:n], scalar1=-5000.0, scalar2=NEG,
               op0=ALU.is_lt, op1=ALU.mult)

    def transposes(i):
        for j0 in range(0, i + 1, 8):
            j1 = min(i + 1, j0 + 8)
            bi = _nxt(k, "pj")
            bank = 6 + (bi % 2)
            pt = k.ps[bank][:].bitcast(BF16)
            for j in range(j0, j1):
                OP(k, "pe", "transpose", [rMB, k.rident], [k.rps[bank]], out=pt[:, (j - j0) * 128:(j - j0 + 1) * 128],
                   in_=MB[:, j * 128:(j + 1) * 128], identity=k.ident[:])
            OP(k, "act", "activation", [k.rps[bank]], [rMBT], out=MBT[:, j0:j1, :],
               in_=pt[:, 0:(j1 - j0) * 128].rearrange("p (j t) -> p j t", t=128), func=AF.Copy)

    def strips(i):
        items = []
        for j in range(i + 1):
            st = {}

            def qk(j=j, st=st):
                ets = []
                for hh in range(2):
                    lb = _nxt(k, "lt") % 4
                    LT = k.ps[lb]
                    LT3 = LT[:].rearrange("p (g t) -> p g t", t=128)
                    OP(k, "pe", "matmul", [rQC, rKC2], [k.rps[lb]], out=LT3,
                       lhsT=KC2[64 * hh:64 * hh + 64, j * 128:(j + 1) * 128],
                       rhs=QC[64 * hh:64 * hh + 64, :, i * 128:(i + 1) * 128], start=True, stop=False)
                    dd = j - i + 15
                    OP(k, "pe", "matmul", [rb], [k.rps[lb]], out=LT3, lhsT=PKD[dd // 8][:, dd % 8, :],
                       rhs=PQD[:, 4 * hh:4 * hh + 4, :], start=False, stop=False)
                    OP(k, "pe", "matmul", [rMBT, k.rident], [k.rps[lb]], out=LT3, lhsT=k.ident[:],
                       rhs=MBT[:, j, :].unsqueeze(1).broadcast_to([128, 4, 128]), start=False, stop=True)
                    ei = _nxt(k, "et") % 4
                    ET, rET = a.ET[ei], a.rET[ei]
                    OP(k, "act", "activation", [k.rps[lb]], [rET], out=ET[:], in_=LT[:], func=AF.Exp)
                    ets.append((ET, rET))
                st["ets"] = ets

            def pv(j=j, st=st):
                for hh in range(2):
                    ET, rET = st["ets"][hh]
                    first = (j == 0 and hh == 0)
                    last = (j == i and hh == 1)
                    OP(k, "pe", "matmul", [rET, rVP], [a.racc], out=k.ps[4][:], lhsT=VP[:, j, hh, :], rhs=ET[:], start=first,
                       stop=last)
                    OP(k, "pe", "matmul", [rET, k.rconst], [a.rden], out=k.ps[5][:], lhsT=k.ONES2[:, hh, :], rhs=ET[:],
                       start=first, stop=last)
            items.append((qk, pv))
        _pipeline(items)

    def normalize(i):
        ni = _nxt(k, "nrm") % 2
        nrm, rn = a.nrm[ni], a.rnrm[ni]
        OP(k, "dve", "reciprocal", [a.rden], [rn], out=nrm[:], in_=k.ps[5][:])
        OP(k, "dve", "tensor_tensor", [a.racc, rn], [a.rattn[g_] for g_ in range(4)], out=a.attnT[:, 0:4, i * 128:(i + 1) * 128],
           in0=k.ps[4][:].rearrange("p (g t) -> p g t", t=128), in1=nrm[:].rearrange("p (g t) -> p g t", t=128), op=ALU.mult)

    for G in groups:
        for i in G:
            indexer(i)
        if G[0] >= 2 and not os.environ.get('DSA_NOBIS'):
            bisect_group(G)
        for b, i in enumerate(G):
            make_mask(i, colof.get(i, b))
            transposes(i)
            if not os.environ.get('DSA_NOSTRIPS'):
                strips(i)
                normalize(i)


def _consts():
    c = np.zeros((128, CONST_COLS), np.float32)
    sr = np.arange(128)[:, None]
    tr = np.arange(128)[None, :]
    c[:, C_CM:C_CM + 128] = np.where(sr <= tr, 0.0, NEG)
    slopes = 2.0 ** (-8.0 * np.arange(1, 9) / 8.0)
    ma = np.zeros((128, 8, 2, 128), np.float32)
    for h in range(8):
        d0 = (tr - sr).astype(np.float32)
        ma[:, h, 0, :] = np.where(sr <= tr, -slopes[h] * d0, NEG)
        d1 = (tr - sr + 128).astype(np.float32)
        ma[:, h, 1, :] = np.where(sr > tr, -slopes[h] * d1, NEG)
    c[:, C_MA:C_MA + 2048] = ma.reshape(128, 2048)
    c[:, C_TRI:C_TRI + 128] = (sr <= tr).astype(np.float32)
    c[:, C_CMQ:C_CMQ + 128] = np.where(tr <= sr, 0.0, -1.0e4)
    pos = np.arange(S, dtype=np.float64)
    inv = 10000.0 ** (-np.arange(16, dtype=np.float64) / 16.0)
    ang = pos[None, :] * inv[:, None]
    for r in range(128):
        d = r % 32
        c[r, C_ROPE:C_ROPE + S] = np.cos(ang[d % 16])
        sg = -1.0 if d < 16 else 1.0
        c[r, C_ROPE + S:C_ROPE + 2 * S] = sg * np.sin(ang[d % 16])
    c[:, C_PQ:C_PQ + 32] = (2.0 ** -(np.arange(32) + 1.0))[None, :]
    pkd = np.zeros((128, 16, 128), np.float32)
    for dd in range(16):
        pkd[0, dd, :] = 128.0 * (dd - 15)
        pkd[1, dd, :] = np.arange(128)
        pkd[2, dd, :] = 1.0
    pqd = np.zeros((128, 8, 128), np.float32)
    for h in range(8):
        pqd[0, h, :] = slopes[h]
        pqd[1, h, :] = slopes[h]
        pqd[2, h, :] = -slopes[h] * np.arange(128)
    c[:, C_PKD:C_PKD + 2048] = pkd.reshape(128, 2048)
    c[:, C_PQD:C_PQD + 1024] = pqd.reshape(128, 1024)
    abi = np.zeros((128, 16, 8), np.float32)
    for dd in range(16):
        for h in range(8):
            abi[:, dd, h] = slopes[h] * (128.0 * (dd - 15) + np.arange(128))
    c[:, C_PK:C_PK + 128] = abi.reshape(128, 128)
    return c
    pk = np.zeros((4, 16, 128), np.float32)
    for dd in range(16):
        pk[0, dd, :] = 128.0 * (dd - 15)
        pk[1, dd, :] = np.arange(128)
        pk[2, dd, :] = 1.0
    pq = np.zeros((4, 8, 128), np.float32)
    for h in range(8):
        pq[0, h, :] = slopes[h]
        pq[1, h, :] = slopes[h]
        pq[2, h, :] = -slopes[h] * np.arange(128)
    c[0:4, C_PK:C_PK + 2048] = pk.reshape(4, 2048)
    c[0:4, C_PQ:C_PQ + 1024] = pq.reshape(4, 1024)
    return c


def kernel(**inputs):
    n = 8
    nseq = 32 // n
    nc = build(nseq)
    shared = {}
    for name in ("even_w_in", "even_b_f", "even_sinks", "odd_w_in", "odd_q_norm", "odd_kv_norm", "odd_w_uq",
                 "odd_w_ukv"):
        shared[name] = np.ascontiguousarray(np.asarray(inputs[name], np.float32)[0])
    for name in ("w_o", "ln_g", "ln_b", "router_w", "router_b", "moe_w_gate", "moe_w_up", "moe_w_down"):
        shared[name] = np.ascontiguousarray(np.asarray(inputs[name], np.float32))
    shared["c_attn"] = _consts()
    x = np.asarray(inputs["x"], np.float32)
    in_maps = []
    for c in range(n):
        m = dict(shared)
        m["x"] = np.ascontiguousarray(x[c * nseq:(c + 1) * nseq])
        in_maps.append(m)
    res = run_bass_kernel_spmd(nc, in_maps, core_ids=list(range(n)))
    return np.concatenate([r["out"] for r in res.results], axis=0)
```
